# Optimizing a Trainium2 kernel written in Bass

```python
import jax
import jax.numpy as jnp
from jax import lax
import numpy as np

D_MODEL = 1024
BATCH = 4
SEQ = 8192
DEPTH = 1

D_MIX = D_MODEL
ATTN_HEAD_DIM = 64
ATTN_HEADS = (D_MIX // 2) // ATTN_HEAD_DIM
ATTN_WIDTH = ATTN_HEADS * ATTN_HEAD_DIM
DILATED_PATTERNS = ((128, 1), (512, 4), (2048, 16))
ALIBI_MAX_BIAS = 8.0
HGRN_KEY_DIM = 128
HGRN_VAL_DIM = 128
HGRN_HEADS = (D_MIX - ATTN_WIDTH) // HGRN_VAL_DIM
HGRN_WIDTH = HGRN_HEADS * HGRN_VAL_DIM
HGRN_FDIM = HGRN_HEADS * HGRN_KEY_DIM
HGRN_CHUNK = 64
IN_SPLITS = (ATTN_WIDTH, ATTN_WIDTH, ATTN_WIDTH, HGRN_FDIM, HGRN_FDIM, HGRN_FDIM, HGRN_WIDTH, HGRN_WIDTH)
D_IN = 3 * ATTN_WIDTH + 3 * HGRN_FDIM + 2 * HGRN_WIDTH
N_EXPERTS = 16
CAPACITY_FACTOR = 2
D_FF_EXPERT = D_MODEL
NORM_EPS = 1e-6
NEG_INF = -1e30

kernel_name = "hybrid_dilated_attn_hgrn2_ecmoe"


def rms_norm(x, w):
    xf = x.astype(jnp.float32)
    y = xf * lax.rsqrt(jnp.mean(xf * xf, axis=-1, keepdims=True) + NORM_EPS)
    return (y * w.astype(jnp.float32)).astype(x.dtype)


def alibi_slopes(n_heads):
    return jnp.exp2(-ALIBI_MAX_BIAS * jnp.arange(1, n_heads + 1, dtype=jnp.float32) / n_heads)


def dilated_window_attention(q, k, v, window, dilation, slopes):
    B, S, H, Dh = q.shape
    half = window // (2 * dilation)
    blk = half
    span = dilation * blk
    Sp = -(-S // span) * span
    L = Sp // dilation
    nb = L // blk

    def to_sub(t):
        t = jnp.pad(t, ((0, 0), (0, Sp - S), (0, 0), (0, 0)))
        return t.reshape(B, L, dilation, H, Dh).transpose(0, 2, 1, 3, 4)

    def windows(t):
        t = jnp.pad(t, ((0, 0), (0, 0), (blk, blk), (0, 0), (0, 0))).reshape(B, dilation, nb + 2, blk, H, Dh)
        return jnp.concatenate([t[:, :, :-2], t[:, :, 1:-1], t[:, :, 2:]], axis=3)

    qs = to_sub(q).reshape(B, dilation, nb, blk, H, Dh)
    ks = windows(to_sub(k))
    vs = windows(to_sub(v))

    qi = jnp.arange(blk)
    kj = jnp.arange(3 * blk)
    delta = kj[None, :] - blk - qi[:, None]
    band = jnp.abs(delta) <= half
    l_k = jnp.arange(nb)[:, None] * blk - blk + kj[None, :]
    pos_k = l_k[None] * dilation + jnp.arange(dilation)[:, None, None]
    key_ok = (l_k[None] >= 0) & (pos_k < S)
    mask = band[None, None, None] & key_ok[:, :, None, None, :]
    bias = -slopes[:, None, None] * (dilation * jnp.abs(delta)).astype(jnp.float32)[None]

    s = jnp.einsum('brnqhd,brnkhd->brnhqk', qs, ks)
    s = jnp.where(mask, s + bias, NEG_INF)
    m = jnp.max(s, axis=-1, keepdims=True)
    e = jnp.exp(s - m)
    den = jnp.sum(e, axis=-1)
    num = jnp.einsum('brnhqk,brnkhd->brnqhd', e, vs)

    def back_stat(t):
        t = t.transpose(0, 1, 2, 4, 3).reshape(B, dilation, L, H)
        return t.transpose(0, 2, 1, 3).reshape(B, Sp, H)[:, :S]

    num = num.reshape(B, dilation, L, H, Dh).transpose(0, 2, 1, 3, 4).reshape(B, Sp, H, Dh)[:, :S]
    return back_stat(m[..., 0]), back_stat(den), num


def dilated_attention_mixer(q, k, v, q_norm_w, k_norm_w):
    B, S, _ = q.shape
    shp = (B, S, ATTN_HEADS, ATTN_HEAD_DIM)
    qh = rms_norm(q.reshape(shp), q_norm_w).astype(jnp.float32) * (ATTN_HEAD_DIM ** -0.5)
    kh = rms_norm(k.reshape(shp), k_norm_w).astype(jnp.float32)
    vh = v.reshape(shp).astype(jnp.float32)
    slopes = alibi_slopes(ATTN_HEADS)
    ms, dens, nums = [], [], []
    for window, dilation in DILATED_PATTERNS:
        m, den, num = dilated_window_attention(qh, kh, vh, window, dilation, slopes)
        ms.append(m)
        dens.append(den)
        nums.append(num)
    m_all = jnp.stack(ms)
    scale = jnp.exp(m_all - jnp.max(m_all, axis=0, keepdims=True))
    numer = jnp.sum(scale[..., None] * jnp.stack(nums), axis=0)
    denom = jnp.sum(scale * jnp.stack(dens), axis=0)
    return (numer / denom[..., None]).reshape(B, S, ATTN_WIDTH)


def chunked_gated_recurrence(q, k, v, log_f):
    B, S, H, Dk = q.shape
    Dv = v.shape[-1]
    C = HGRN_CHUNK
    n = S // C

    def to_chunks(t):
        return t.reshape(B, n, C, H, t.shape[-1]).transpose(1, 0, 3, 2, 4)

    incl = jnp.tril(jnp.ones((C, C), dtype=bool))[:, :, None]

    def step(state, inp):
        qb, kb, vb, gb = inp
        b = jnp.cumsum(gb, axis=2)
        o_inter = jnp.einsum('bhck,bhkv->bhcv', qb * jnp.exp(b), state)
        diff = b[:, :, :, None, :] - b[:, :, None, :, :]
        decay = jnp.exp(jnp.where(incl, diff, -jnp.inf))
        a = jnp.einsum('bhtk,bhsk,bhtsk->bhts', qb, kb, decay)
        o_intra = jnp.einsum('bhts,bhsv->bhtv', a, vb)
        b_last = b[:, :, -1:, :]
        k_dec = kb * jnp.exp(b_last - b)
        state = jnp.exp(b_last[:, :, 0, :])[..., None] * state + jnp.einsum('bhsk,bhsv->bhkv', k_dec, vb)
        return state, o_inter + o_intra

    init = jnp.zeros((B, H, Dk, Dv), jnp.float32)
    _, out = lax.scan(step, init, (to_chunks(q), to_chunks(k), to_chunks(v), to_chunks(log_f)))
    return out.transpose(1, 0, 3, 2, 4).reshape(B, S, H, Dv)


def hgrn2_mixer(q, f_fwd, f_bwd, i, g, lb_fwd, lb_bwd, out_norm_w):
    B, S, _ = q.shape

    def heads(t, d):
        return t.reshape(B, S, HGRN_HEADS, d).astype(jnp.float32)

    qh = jax.nn.silu(heads(q, HGRN_KEY_DIM))
    vh = heads(i, HGRN_VAL_DIM)

    def gates(f_raw, lb):
        lb = lb.reshape(HGRN_HEADS, HGRN_KEY_DIM)
        z = heads(f_raw, HGRN_KEY_DIM)
        f = lb + (1.0 - lb) * jax.nn.sigmoid(z)
        return (1.0 - lb) * jax.nn.sigmoid(-z), jnp.log(f)

    k_f, lf_f = gates(f_fwd, lb_fwd)
    k_b, lf_b = gates(f_bwd, lb_bwd)
    o_f = chunked_gated_recurrence(qh, k_f, vh, lf_f)
    flip = lambda t: jnp.flip(t, axis=1)
    o_b = flip(chunked_gated_recurrence(flip(qh), flip(k_b), flip(vh), flip(lf_b)))
    o = rms_norm(o_f + o_b, out_norm_w) * jax.nn.silu(heads(g, HGRN_VAL_DIM))
    return o.reshape(B, S, HGRN_WIDTH)


def expert_choice_moe(h, w_router, w_gate, w_up, w_down):
    B, S, D = h.shape
    cap = max(1, CAPACITY_FACTOR * S // N_EXPERTS)
    logits = jnp.einsum('bsd,de->bse', h, w_router).astype(jnp.float32)
    aff = jax.nn.softmax(logits, axis=-1)
    gate, idx = lax.top_k(aff.transpose(0, 2, 1), cap)
    xin = jax.vmap(lambda hb, ib: hb[ib])(h, idx)
    hid = jax.nn.silu(jnp.einsum('becd,edf->becf', xin, w_gate)) * jnp.einsum('becd,edf->becf', xin, w_up)
    y = jnp.einsum('becf,efd->becd', hid, w_down) * gate[..., None].astype(h.dtype)
    bidx = jnp.arange(B)[:, None, None]
    return jnp.zeros_like(h).at[bidx, idx].add(y)


def setup_inputs(seed: int = 0) -> dict:
    key = jax.random.key(seed)
    ks = jax.random.split(key, 16)
    f32 = jnp.float32
    nrm = lambda k, shape, fan_in: jax.random.normal(k, shape, f32) * (fan_in ** -0.5)
    gain = lambda k, shape: 1.0 + 0.02 * jax.random.normal(k, shape, f32)
    return {
        "x": jax.random.normal(ks[0], (BATCH, SEQ, D_MODEL), f32),
        "norm1_w": gain(ks[1], (DEPTH, D_MODEL)),
        "w_in": nrm(ks[2], (DEPTH, D_MODEL, D_IN), D_MODEL),
        "attn_q_norm_w": gain(ks[3], (DEPTH, ATTN_HEAD_DIM)),
        "attn_k_norm_w": gain(ks[4], (DEPTH, ATTN_HEAD_DIM)),
        "hgrn_lb_fwd": 1.0 + 0.1 * jax.random.normal(ks[5], (DEPTH + 1, HGRN_FDIM), f32),
        "hgrn_lb_bwd": 1.0 + 0.1 * jax.random.normal(ks[6], (DEPTH + 1, HGRN_FDIM), f32),
        "hgrn_out_norm_w": gain(ks[7], (DEPTH, HGRN_VAL_DIM)),
        "w_out": nrm(ks[8], (DEPTH, D_MIX, D_MODEL), D_MIX),
        "norm2_w": gain(ks[9], (DEPTH, D_MODEL)),
        "w_router": nrm(ks[10], (DEPTH, D_MODEL, N_EXPERTS), D_MODEL),
        "w_expert_gate": nrm(ks[11], (DEPTH, N_EXPERTS, D_MODEL, D_FF_EXPERT), D_MODEL),
        "w_expert_up": nrm(ks[12], (DEPTH, N_EXPERTS, D_MODEL, D_FF_EXPERT), D_MODEL),
        "w_expert_down": nrm(ks[13], (DEPTH, N_EXPERTS, D_FF_EXPERT, D_MODEL), D_FF_EXPERT),
    }


def reference(x, norm1_w, w_in, attn_q_norm_w, attn_k_norm_w, hgrn_lb_fwd, hgrn_lb_bwd,
              hgrn_out_norm_w, w_out, norm2_w, w_router, w_expert_gate, w_expert_up, w_expert_down):
    lb_f_all = jnp.cumsum(jax.nn.softmax(hgrn_lb_fwd.astype(jnp.float32), axis=0), axis=0)
    lb_b_all = jnp.cumsum(jax.nn.softmax(hgrn_lb_bwd.astype(jnp.float32), axis=0), axis=0)
    split_at = np.cumsum(IN_SPLITS)[:-1].tolist()
    for l in range(DEPTH):
        h = rms_norm(x, norm1_w[l])
        proj = jnp.einsum('bsd,de->bse', h, w_in[l])
        aq, ak, av, hq, hf_f, hf_b, hi, hg = jnp.split(proj, split_at, axis=-1)
        a_out = dilated_attention_mixer(aq, ak, av, attn_q_norm_w[l], attn_k_norm_w[l])
        b_out = hgrn2_mixer(hq, hf_f, hf_b, hi, hg, lb_f_all[l], lb_b_all[l], hgrn_out_norm_w[l])
        mixed = jnp.concatenate([a_out.astype(x.dtype), b_out.astype(x.dtype)], axis=-1)
        x = x + jnp.einsum('bsm,md->bsd', mixed, w_out[l])
        x = x + expert_choice_moe(rms_norm(x, norm2_w[l]), w_router[l], w_expert_gate[l],
                                  w_expert_up[l], w_expert_down[l])
    return x
```

```python
from contextlib import ExitStack
import numpy as np
import concourse.bass as bass
import concourse.mybir as mybir
from concourse.bass_utils import run_bass_kernel_spmd

F32 = mybir.dt.float32
BF16 = mybir.dt.bfloat16
U32 = mybir.dt.uint32
AF = mybir.ActivationFunctionType
ALU = mybir.AluOpType
AX = mybir.AxisListType

S = 8192
D = 1024
PAD = 1024
EPS = 1e-6


class Tok:
    __slots__ = ("name", "last_w", "readers", "sem", "dma_cnt", "last_dma", "phys")

    def __init__(self, name):
        self.name = name
        self.last_w = None
        self.readers = []
        self.sem = None
        self.dma_cnt = 0
        self.last_dma = None
        self.phys = None


class Op:
    __slots__ = ("eng", "fn", "deps", "is_dma", "chan", "chan_ord", "ticket", "needs_inc", "idx", "phys")


class PhysSem:
    __slots__ = ("count", "sem")

    def __init__(self):
        self.count = 0
        self.sem = None


ENGS = ("pe", "act", "dve", "pool", "sp")


class Prog:
    def __init__(self, nc):
        self.nc = nc
        self.ops = []
        self.toks = {}
        self.last_op = {e: None for e in ENGS}
        self.chans = []
        self.phys_all = []
        self.phys_free = []
        self.sw_chans = []

    def tok(self, name):
        t = self.toks.get(name)
        if t is None:
            t = Tok(name)
            self.toks[name] = t
        return t

    def _T(self, xs):
        if isinstance(xs, (str, Tok)):
            xs = [xs]
        return [self.tok(x) if isinstance(x, str) else x for x in xs]

    def _add(self, eng, fn, reads, writes, is_dma, chan):
        op = Op()
        op.eng = eng
        op.fn = fn
        op.is_dma = is_dma
        op.chan = chan
        op.ticket = None
        op.needs_inc = is_dma
        op.idx = len(self.ops)
        deps = {}

        def need(d, kind):
            if d is op:
                return
            if not is_dma and not d.is_dma and d.eng == eng:
                if eng == "pe":
                    return
            deps[d.idx] = d

        for t in self._T(reads):
            if t.last_w is not None:
                need(t.last_w, "raw")
            if t.name.startswith("bank"):
                for r in t.readers:
                    if r.eng != eng:
                        need(r, "rar")
            t.readers.append(op)
        for t in self._T(writes):
            if t.last_w is not None:
                need(t.last_w, "waw")
            for r in t.readers:
                need(r, "war")
            t.last_w = op
            t.readers = []
        op.deps = list(deps.values())
        if is_dma:
            if chan.phys is None:
                if self.phys_free and eng != "pool":
                    chan.phys = self.phys_free.pop()
                else:
                    chan.phys = PhysSem()
                    self.phys_all.append(chan.phys)
                if eng == "pool":
                    self.sw_chans.append(chan)
                else:
                    self.chans.append(chan)
            chan.phys.count += 1
            op.phys = chan.phys
            op.chan_ord = chan.phys.count
            chan.last_dma = op
        self.ops.append(op)
        self.last_op[eng] = op
        return op

    def op(self, eng, fn, reads=(), writes=()):
        return self._add(eng, fn, reads, writes, False, None)

    def dma(self, eng, fn, reads=(), writes=(), chan=None):
        chan = self.tok(chan) if isinstance(chan, str) else chan
        return self._add(eng, fn, reads, writes, True, chan)

    def barrier(self):
        lasts = [o for o in self.last_op.values() if o is not None and not o.is_dma]
        dmas = [c.last_dma for c in self.chans] + [c.last_dma for c in self.sw_chans]
        for e in ENGS:
            op = Op()
            op.eng = e
            op.fn = None
            op.is_dma = False
            op.chan = None
            op.ticket = None
            op.needs_inc = False
            op.idx = len(self.ops)
            op.deps = [o for o in lasts if o.eng != e] + dmas
            self.ops.append(op)
        for t in self.toks.values():
            t.last_w = None
            t.readers = []
        for c in self.chans:
            self.phys_free.append(c.phys)
            c.phys = None
        self.chans = []

    def emit(self, stack):
        nc = self.nc
        for op in self.ops:
            for d in op.deps:
                if not d.is_dma:
                    d.needs_inc = True
        eng_sem = {}
        counts = {e: 0 for e in ENGS}
        for e in ENGS:
            eng_sem[e] = stack.enter_context(nc.semaphore("cs_" + e))
        nch = len(self.phys_all)
        for i, ph in enumerate(self.phys_all):
            ph.sem = stack.enter_context(nc.semaphore("ch%d" % i))
        for op in self.ops:
            if op.is_dma:
                pass
            elif op.needs_inc:
                counts[op.eng] += 1
                op.ticket = counts[op.eng]
        self.counts = counts
        self.nch = nch
        block = stack.enter_context(nc.Block())

        def run(e, eng):
            seen = {}
            for op in self.ops:
                if op.eng != e:
                    continue
                for d in op.deps:
                    if d.is_dma:
                        sem, val = d.phys.sem, 16 * d.chan_ord
                    else:
                        sem, val = eng_sem[d.eng], d.ticket
                    key = id(sem)
                    if seen.get(key, 0) < val:
                        eng.wait_ge(sem, val)
                        seen[key] = val
                if op.fn is None:
                    continue
                inst = op.fn(eng)
                if op.is_dma:
                    inst.then_inc(op.phys.sem, 16)
                elif op.needs_inc:
                    inst.then_inc(eng_sem[e], 1)

        @block.tensor
        def _(eng):
            run("pe", eng)

        @block.scalar
        def _(eng):
            run("act", eng)

        @block.vector
        def _(eng):
            run("dve", eng)

        @block.gpsimd
        def _(eng):
            run("pool", eng)

        @block.sync
        def _(eng):
            run("sp", eng)


class Arena:
    def __init__(self, ap, nwords):
        self.ap = ap
        self.n = nwords
        self.off = 0

    def reset(self):
        self.off = 0

    def alloc(self, shape, dt):
        free = 1
        for s in shape[1:]:
            free *= s
        words = free if dt in (F32, U32, mybir.dt.int32) else (free + 1) // 2
        words = (words + 7) // 8 * 8
        assert self.off + words <= self.n, ("SBUF arena overflow", self.off, words, self.n)
        v = self.ap[0:shape[0], self.off:self.off + words]
        self.off += words
        if dt != F32:
            v = v.bitcast(dt)
        v = v[:, 0:free]
        if len(shape) == 3:
            v = v.rearrange("p (a b) -> p a b", a=shape[1])
        elif len(shape) == 4:
            v = v.rearrange("p (a b c) -> p a b c", a=shape[1], b=shape[2])
        return v


class Rot:
    def __init__(self, items):
        self.items = items
        self.i = 0

    def next(self):
        it = self.items[self.i % len(self.items)]
        self.i += 1
        return it


def build_nc(stop_after="F", dbg=(), nst=None, skip=""):
    nc = bass.Bass("TRN2", target_bir_lowering=False)

    def din(name, shape, dt=F32):
        return nc.dram_tensor(name, list(shape), dt, kind="ExternalInput").ap()

    def dscr(name, shape, dt):
        kind = "ExternalOutput" if name in dbg else "Internal"
        return nc.dram_tensor(name, list(shape), dt, kind=kind).ap()

    x = din("x", [S, D])
    w_in = din("w_in", [D, 4096])
    n1w = din("n1w", [1, D])
    wq2 = din("wq2", [128, 2])
    lbs = din("lbs", [128, 2, 2, 4])
    onw = din("onw", [1, 128])
    w_out = din("w_out", [D, D])
    n2w = din("n2w", [1, D])
    w_r = din("w_r", [D, 16])
    w_g = din("w_g", [16, D, D])
    w_u = din("w_u", [16, D, D])
    w_d = din("w_d", [16, D, D])
    emask = din("emask", [128, 24, 256])
    hmask = din("hmask", [128, 2, 128])
    tokab = din("tokab", [128, 32, 3])
    iot = din("iot", [128, 1024])
    dum = din("dum", [128, 2])
    out = nc.dram_tensor("out", [S // 2 + 128, D], F32, kind="ExternalOutput").ap()

    QT = dscr("QT", [4, 128, S], BF16)
    KT = dscr("KT", [4, 128, S + 2 * PAD], BF16)
    VA = dscr("VA", [S + 2 * PAD, 520], BF16)
    QB = dscr("QB", [2, 4, 128, S], BF16)
    KB = dscr("KB", [2, 4, 128, S], BF16)
    KD = dscr("KD", [2, 4, 128, 64, 128], BF16)
    HV = dscr("HV", [4, 128, 64, 128], BF16)
    GS = dscr("GS", [4, 128, 64, 128], BF16)
    EBL = dscr("EBL", [2, 4, 128, 128], F32)
    MXT = dscr("MXT", [8, 128, S], BF16)
    X1 = dscr("X1", [S, D], F32)
    H2 = dscr("H2", [S // 2 + 128, D], BF16)

    P = Prog(nc)
    with ExitStack() as st:
        NW = 53000
        arena_t = st.enter_context(nc.sbuf_tensor("arena", [128, NW], F32))
        A = Arena(arena_t, NW)
        psum_t = [st.enter_context(nc.psum_tensor("ps%d" % i, [128, 512], F32)) for i in range(8)]

        def psv(bank, shape, dt=F32, half=None):
            v = psum_t[bank][:, :]
            if half is not None:
                v = v[:, half * 256:(half + 1) * 256]
            if dt != F32:
                v = v.bitcast(dt)
            free = 1
            for s_ in shape[1:]:
                free *= s_
            v = v[:, 0:free]
            if len(shape) == 3:
                v = v.rearrange("p (a b) -> p a b", a=shape[1])
            return v[0:shape[0]]

        ident = A.alloc([128, 128], BF16)
        identf = A.alloc([128, 128], F32)
        P.op("pool", lambda e: e.memset(identf, 0.0), writes=["identf"])
        P.op("pool", lambda e: e.affine_select(out=identf, in_=identf, pattern=[[-1, 128]], compare_op=ALU.not_equal,
                                               fill=1.0, base=0, channel_multiplier=1), reads=["identf"], writes=["identf"])
        P.op("dve", lambda e: e.tensor_copy(out=ident, in_=identf), reads=["identf"], writes=["ident"])
        zeros = A.alloc([128, 2048], BF16)
        P.op("pool", lambda e: e.memset(zeros, 0.0), writes=["zeros"])
        const_end = A.off

        TS = 256
        NST = nst or (S // TS)
        WIN = A.alloc([128, 8, 4096], BF16)
        wn = A.alloc([128, D], F32)
        wqs = A.alloc([128, 2], F32)
        lbt = A.alloc([128, 2, 2, 4], F32)
        lb = A.alloc([128, 2, 4], F32)
        oml = A.alloc([128, 2, 4], F32)
        bones = A.alloc([128, 128], BF16)
        rmask = A.alloc([128, TS], F32)
        stage = [A.alloc([128, 1024], F32) for _ in range(2)]

        P.dma("sp", lambda e: e.dma_start(out=wn, in_=n1w.partition_broadcast(128)), writes=["wn"], chan="wn")
        P.dma("sp", lambda e: e.dma_start(out=wqs, in_=wq2), writes=["wqs"], chan="wqs")
        P.dma("sp", lambda e: e.dma_start(out=lbt, in_=lbs), writes=["lbt"], chan="lbt")
        P.op("dve", lambda e: e.tensor_scalar(out=wqs[:, 0:1], in0=wqs[:, 0:1], scalar1=0.125, scalar2=None, op0=ALU.mult),
             reads=["wqs"], writes=["wqs"])
        P.op("dve", lambda e: e.tensor_tensor(out=lb, in0=lbt[:, :, 1, :], in1=lbt[:, :, 0, :], op=ALU.subtract), reads=["lbt"], writes=["lb"])
        P.op("act", lambda e: e.activation(out=lb, in_=lb, func=AF.Exp), reads=["lb"], writes=["lb"])
        P.op("dve", lambda e: e.tensor_scalar(out=lb, in0=lb, scalar1=1.0, scalar2=None, op0=ALU.add), reads=["lb"], writes=["lb"])
        P.op("dve", lambda e: e.reciprocal(out=lb, in_=lb), reads=["lb"], writes=["lb"])
        P.op("dve", lambda e: e.tensor_scalar(out=oml, in0=lb, scalar1=-1.0, scalar2=1.0, op0=ALU.mult, op1=ALU.add), reads=["lb"], writes=["oml"])
        P.op("pool", lambda e: e.memset(bones, 0.0), writes=["bones"])
        P.op("pool", lambda e: e.memset(bones[0:64, 0:64], 1.0), reads=["bones"], writes=["bones"])
        P.op("pool", lambda e: e.memset(bones[64:128, 64:128], 1.0), reads=["bones"], writes=["bones"])
        P.op("pool", lambda e: e.memset(rmask, 1.0), writes=["rmask"])
        P.op("pool", lambda e: e.memset(rmask.rearrange("p (c t) -> p c t", t=64)[:, :, 0:1], 0.0), reads=["rmask"], writes=["rmask"])
        for c in range(4):
            for off in (0, PAD + S):
                P.dma("sp", lambda e, c=c, off=off: e.dma_start(out=KT[c, :, off:off + PAD], in_=zeros[:, 0:PAD]),
                      reads=["zeros"], writes=["KTpad"], chan="zp%d_%d" % (c, off))
        for off in (0, PAD + S):
            for j in range(8):
                P.dma("sp", lambda e, off=off, j=j: e.dma_start(out=VA[off + j * 128:off + (j + 1) * 128, :], in_=zeros[:, 0:520]),
                      reads=["zeros"], writes=["VApad"], chan="zv%d_%d" % (off, j))
        for k in range(8):
            for hcol in range(4):
                sg, sgn = stage[hcol % 2], "stage%d" % (hcol % 2)
                P.dma("sp", lambda e, k=k, hcol=hcol, sg=sg: e.dma_start(out=sg, in_=w_in[k * 128:(k + 1) * 128, hcol * 1024:(hcol + 1) * 1024]),
                      writes=[sgn], chan=sgn)
                eng = ("pool", "dve", "act", "dve")[hcol]
                if eng == "act":
                    P.op("act", lambda e, k=k, hcol=hcol, sg=sg: e.copy(out=WIN[:, k, hcol * 1024:(hcol + 1) * 1024], in_=sg), reads=[sgn], writes=["WIN"])
                else:
                    P.op(eng, lambda e, k=k, hcol=hcol, sg=sg: e.tensor_copy(out=WIN[:, k, hcol * 1024:(hcol + 1) * 1024], in_=sg), reads=[sgn], writes=["WIN"])

        NB = 2
        xt = [A.alloc([128, 2, D], F32) for _ in range(NB)]
        junk = A.alloc([128, D], BF16)
        ss = [A.alloc([128, 2], F32) for _ in range(NB)]
        xn = A.alloc([128, 2, D], BF16)
        hT = [A.alloc([128, 8, TS], BF16) for _ in range(NB)]
        qk_st = [A.alloc([128, 8, TS], BF16) for _ in range(NB)]
        va_st = [A.alloc([128, 2, 8, 65], BF16) for _ in range(NB)]
        qb_st = [A.alloc([128, 2, 4, TS], BF16)] * NB
        kb_st = [A.alloc([128, 2, 4, TS], BF16)] * NB
        kd_st = [A.alloc([128, 2, 2, 512], BF16)] * NB
        hv_st = [A.alloc([128, 2, 512], BF16) for _ in range(NB)]
        gs_st = [A.alloc([128, 2, 512], BF16) for _ in range(NB)]
        ebl_st = [A.alloc([128, 2, 4, 4], F32) for _ in range(NB)]
        qs_t = A.alloc([128, 4, TS], F32)
        NTMP = 4
        NT5 = 5
        sqb = [A.alloc([128, TS], BF16) for _ in range(NT5)]
        rr = [A.alloc([128, TS], F32) for _ in range(NT5)]
        t_q = [A.alloc([128, TS], F32) for _ in range(NT5)]
        t_e = [A.alloc([128, TS], F32) for _ in range(NTMP)]
        t_f = [A.alloc([128, TS], F32) for _ in range(NTMP)]
        t_lf = [A.alloc([128, TS], F32) for _ in range(NTMP)]
        t_k = [A.alloc([128, TS], F32) for _ in range(NTMP)]
        t_b = [A.alloc([128, TS], F32) for _ in range(NTMP)]
        t_eb = [A.alloc([128, TS], F32) for _ in range(NTMP)]
        t_enb = [A.alloc([128, TS], F32) for _ in range(NTMP)]
        t_kbf = [A.alloc([128, TS], F32) for _ in range(NTMP)]
        t_kdT = [A.alloc([128, TS], BF16) for _ in range(9)]
        t_g = [A.alloc([128, 512], F32) for _ in range(2)]
        for i in range(NB):
            P.op("pool", lambda e, i=i: e.memset(va_st[i], 1.0), writes=["va_st%d" % i])

        psF = Rot([(psv(b_, [128, TS]), "bank%d" % b_) for b_ in (1, 2, 3, 7)])
        DEFER = 3
        pend = []

        def defer(fn_):
            pend.append(fn_)
            while len(pend) > DEFER:
                pend.pop(0)()

        def flush():
            while pend:
                pend.pop(0)()
        psTm = Rot([(psv(4 + i, [128, 512]), "bank%d" % (4 + i)) for i in range(2)])
        psM = Rot([(psv(6, [128, TS]), "bank6")])
        psK = Rot([(psv(6, [128, 2, 128], BF16), "bank6")])
        tmp_i = [0]

        def load_x(stI):
            bi = stI % NB
            t0 = stI * TS
            P.dma("sp", lambda e, bi=bi, t0=t0: e.dma_start(out=xt[bi], in_=x[t0:t0 + TS, :].rearrange("(s p) d -> p s d", p=128)),
                  writes=["xt%d" % bi], chan="xt%d" % bi)

        def prep_tile(stI):
            bi = stI % NB
            t0 = stI * TS
            X_, H_, SS_ = "xt%d" % bi, "hT%d" % bi, "ss%d" % bi
            for s in range(2):
                P.op("act", lambda e, bi=bi, s=s: e.activation(out=junk, in_=xt[bi][:, s, :], func=AF.Square, accum_out=ss[bi][:, s:s + 1]),
                     reads=[X_], writes=["junk", SS_])
            P.op("act", lambda e, bi=bi: e.activation(out=ss[bi], in_=ss[bi], func=AF.Ln, scale=1.0 / D, bias=EPS), reads=[SS_], writes=[SS_])
            P.op("act", lambda e, bi=bi: e.activation(out=ss[bi], in_=ss[bi], func=AF.Exp, scale=-0.5), reads=[SS_], writes=[SS_])
            for s in range(2):
                P.op("dve", lambda e, bi=bi, s=s: e.scalar_tensor_tensor(out=xn[:, s, :], in0=xt[bi][:, s, :], scalar=ss[bi][:, s:s + 1],
                                                                      in1=wn, op0=ALU.mult, op1=ALU.mult),
                     reads=[X_, SS_, "wn"], writes=["xn%d" % s])

        def prep_tile_b(stI):
            bi = stI % NB
            H_ = "hT%d" % bi
            for s in range(2):
                pT = psv(0, [128, 8, 128], BF16)
                for c in range(8):
                    P.op("pe", lambda e, s=s, c=c, pT=pT: e.transpose(out=pT[:, c, :], in_=xn[:, s, c * 128:(c + 1) * 128], identity=ident),
                         reads=["xn%d" % s, "ident"], writes=["bank0"])
                P.op("act", lambda e, bi=bi, s=s, pT=pT: e.copy(out=hT[bi][:, :, s * 128:(s + 1) * 128], in_=pT),
                     reads=["bank0"], writes=[H_])

        kdp = []
        late_q = []

        def do_tile(stI):
            bi = stI % NB
            t0 = stI * TS
            X_, H_, SS_ = "xt%d" % bi, "hT%d" % bi, "ss%d" % bi

            if stI + 1 < NST:
                prep_tile(stI + 1)
            if stI + 2 < NST:
                load_x(stI + 2)

            def fm_chunk(col0):
                ps, pn = psF.next()
                for k in range(8):
                    P.op("pe", lambda e, k=k, ps=ps: e.matmul(ps, lhsT=WIN[:, k, col0:col0 + 128], rhs=hT[bi][:, k, :], start=(k == 0), stop=(k == 7)),
                         reads=["WIN", H_], writes=[pn])
                return ps, pn

            VA_, HV_, GS_ = "va_st%d" % bi, "hv_st%d" % bi, "gs_st%d" % bi

            def tm_group(s):
                for gi, col0 in enumerate((1024, 3072, 3584)):
                    ps, pn = psTm.next()
                    for k in range(8):
                        P.op("pe", lambda e, k=k, ps=ps, s=s, col0=col0: e.matmul(ps, lhsT=hT[bi][:, k, s * 128:(s + 1) * 128], rhs=WIN[:, k, col0:col0 + 512],
                                                                              start=(k == 0), stop=(k == 7)), reads=["WIN", H_], writes=[pn])
                    if gi == 0:
                        P.op("act", lambda e, ps=ps, s=s: e.copy(out=va_st[bi][:, s, :, 0:64], in_=ps.rearrange("p (h d) -> p h d", h=8)), reads=[pn], writes=[VA_])
                    elif gi == 1:
                        P.op("dve", lambda e, ps=ps, s=s: e.tensor_copy(out=hv_st[bi][:, s, :], in_=ps), reads=[pn], writes=[HV_])
                    else:
                        tg, tgn = t_g[s], "t_g%d" % s
                        P.op("act", lambda e, ps=ps, tg=tg: e.activation(out=tg, in_=ps, func=AF.Exp, scale=-1.0), reads=[pn], writes=[tgn])
                        P.op("act", lambda e, tg=tg: e.activation(out=tg, in_=tg, func=AF.Ln, bias=1.0), reads=[tgn], writes=[tgn])
                        P.op("act", lambda e, tg=tg: e.activation(out=tg, in_=tg, func=AF.Exp, scale=-1.0), reads=[tgn], writes=[tgn])
                        P.op("dve", lambda e, ps=ps, tg=tg, s=s: e.tensor_tensor(out=gs_st[bi][:, s, :], in0=ps, in1=tg, op=ALU.mult), reads=[pn, tgn], writes=[GS_])

            QK_ = "qk_st%d" % bi
            for ci in range(8):
                ps, pn = fm_chunk(ci * 128)
                ti = tmp_i[0] % NT5
                tmp_i[0] += 1
                wcol = 0 if ci < 4 else 1
                P.op("act", lambda e, ps=ps, ti=ti: e.activation(out=sqb[ti], in_=ps, func=AF.Square), reads=[pn], writes=["sqb%d" % ti])
                P.op("dve", lambda e, ps=ps, ti=ti: e.tensor_copy(out=t_q[ti], in_=ps), reads=[pn, "sqb%d" % ti], writes=["t_q%d" % ti])

                def qk_tail(ti=ti, ci=ci, wcol=wcol):
                    pm, pmn = psM.next()
                    P.op("pe", lambda e: e.matmul(pm, lhsT=bones, rhs=sqb[ti], start=True, stop=True), reads=["bones", "sqb%d" % ti], writes=[pmn])
                    P.op("act", lambda e: e.activation(out=rr[ti], in_=pm, func=AF.Ln, scale=1.0 / 64, bias=EPS), reads=[pmn], writes=["rr%d" % ti])
                    P.op("act", lambda e: e.activation(out=rr[ti], in_=rr[ti], func=AF.Exp, scale=-0.5), reads=["rr%d" % ti], writes=["rr%d" % ti])
                    P.op("dve", lambda e: e.scalar_tensor_tensor(out=qk_st[bi][:, ci, :], in0=t_q[ti], scalar=wqs[:, wcol:wcol + 1], in1=rr[ti], op0=ALU.mult, op1=ALU.mult),
                         reads=["t_q%d" % ti, "wqs", "rr%d" % ti], writes=[QK_])
                defer(qk_tail)
            while late_q:
                late_q.pop(0)()
            if stI + 1 < NST:
                prep_tile_b(stI + 1)
            H4 = range(4)
            for h in H4:
                ps, pn = fm_chunk(1536 + h * 128)
                P.op("act", lambda e, ps=ps, h=h: e.activation(out=t_e[h], in_=ps, func=AF.Exp, scale=-1.0), reads=[pn], writes=["t_e%d" % h])
                P.op("dve", lambda e, ps=ps, h=h: e.tensor_copy(out=qs_t[:, h, :], in_=ps), reads=[pn, "t_e%d" % h], writes=["qs%d" % h])
            flush()
            P.dma("sp", lambda e, bi=bi, t0=t0: e.dma_start(out=QT[:, :, t0:t0 + TS].rearrange("c p t -> p c t"), in_=qk_st[bi][:, 0:4, :]),
                  reads=[QK_], writes=["QT"], chan=QK_ + "q")
            P.dma("sp", lambda e, bi=bi, t0=t0: e.dma_start(out=KT[:, :, PAD + t0:PAD + t0 + TS].rearrange("c p t -> p c t"), in_=qk_st[bi][:, 4:8, :]),
                  reads=[QK_], writes=["KT"], chan=QK_ + "k")
            tm_group(0)
            for h in H4:
                P.op("act", lambda e, h=h: e.activation(out=t_e[h], in_=t_e[h], func=AF.Ln, bias=1.0), reads=["t_e%d" % h], writes=["t_e%d" % h])
            for h in H4:
                P.op("act", lambda e, h=h: e.activation(out=t_e[h], in_=t_e[h], func=AF.Exp, scale=-1.0), reads=["t_e%d" % h], writes=["t_e%d" % h])
            for h in H4:
                P.op("dve", lambda e, h=h: e.tensor_tensor(out=qs_t[:, h, :], in0=qs_t[:, h, :], in1=t_e[h], op=ALU.mult), reads=["qs%d" % h, "t_e%d" % h], writes=["qs%d" % h])

            QB_, KB_, KD_, EB_ = "qb_st", "kb_st", "kd_st", "ebl_st%d" % bi
            for dr in range(2):
                nm = lambda s_, h: "%s%d" % (s_, h)
                for h in H4:
                    ps, pn = fm_chunk(2048 + dr * 512 + h * 128)
                    P.op("act", lambda e, ps=ps, h=h: e.activation(out=t_e[h], in_=ps, func=AF.Exp, scale=-1.0), reads=[pn], writes=[nm("t_e", h)])
                if dr == 0:
                    tm_group(1)
                else:
                    while kdp:
                        kdp.pop(0)()
                for h in H4:
                    P.op("act", lambda e, h=h: e.activation(out=t_e[h], in_=t_e[h], func=AF.Ln, bias=1.0), reads=[nm("t_e", h)], writes=[nm("t_e", h)])
                for h in H4:
                    P.op("act", lambda e, h=h: e.activation(out=t_e[h], in_=t_e[h], func=AF.Exp, scale=-1.0), reads=[nm("t_e", h)], writes=[nm("t_e", h)])
                for h in H4:
                    P.op("dve", lambda e, h=h, dr=dr: e.tensor_scalar(out=t_f[h], in0=t_e[h], scalar1=oml[:, dr, h:h + 1], scalar2=lb[:, dr, h:h + 1], op0=ALU.mult, op1=ALU.add),
                         reads=[nm("t_e", h), "oml", "lb"], writes=[nm("t_f", h)])
                for h in H4:
                    P.op("act", lambda e, h=h: e.activation(out=t_lf[h], in_=t_f[h], func=AF.Ln), reads=[nm("t_f", h)], writes=[nm("t_lf", h)])
                    P.op("pool", lambda e, h=h: e.tensor_scalar(out=t_k[h], in0=t_f[h], scalar1=-1.0, scalar2=1.0, op0=ALU.mult, op1=ALU.add), reads=[nm("t_f", h)], writes=[nm("t_k", h)])
                for h in H4:
                    P.op("dve", lambda e, h=h: e.tensor_tensor_scan(out=t_b[h], data0=rmask, data1=t_lf[h], initial=0.0, op0=ALU.mult, op1=ALU.add),
                         reads=["rmask", nm("t_lf", h)], writes=[nm("t_b", h)])
                if dr == 1:
                    for h in H4:
                        b3 = t_b[h].rearrange("p (c t) -> p c t", t=64)
                        P.op("dve", lambda e, h=h: e.tensor_tensor(out=t_e[h], in0=t_lf[h], in1=t_b[h], op=ALU.subtract), reads=[nm("t_lf", h), nm("t_b", h)], writes=[nm("t_e", h)])
                        P.op("dve", lambda e, h=h, b3=b3: e.tensor_tensor(out=b3, in0=t_e[h].rearrange("p (c t) -> p c t", t=64), in1=b3[:, :, 63:64].to_broadcast([128, 4, 64]), op=ALU.add),
                             reads=[nm("t_e", h), nm("t_b", h)], writes=[nm("t_b", h)])
                for h in H4:
                    P.op("act", lambda e, h=h: e.activation(out=t_eb[h], in_=t_b[h], func=AF.Exp), reads=[nm("t_b", h)], writes=[nm("t_eb", h)])
                for h in H4:
                    P.op("act", lambda e, h=h: e.activation(out=t_enb[h], in_=t_b[h], func=AF.Exp, scale=-1.0), reads=[nm("t_b", h)], writes=[nm("t_enb", h)])
                for h in H4:
                    P.op("pool", lambda e, h=h, dr=dr: e.tensor_tensor(out=qb_st[bi][:, dr, h, :], in0=qs_t[:, h, :], in1=t_eb[h], op=ALU.mult), reads=["qs%d" % h, nm("t_eb", h)], writes=[QB_])
                    P.op("pool", lambda e, h=h: e.tensor_tensor(out=t_kbf[h], in0=t_k[h], in1=t_enb[h], op=ALU.mult), reads=[nm("t_k", h), nm("t_enb", h)], writes=[nm("t_kbf", h)])
                col = 63 if dr == 0 else 0
                for h in H4:
                    tj = tmp_i[0] % 9
                    tmp_i[0] += 1
                    eb3 = t_eb[h].rearrange("p (c t) -> p c t", t=64)
                    P.op("dve", lambda e, h=h, dr=dr: e.tensor_copy(out=kb_st[bi][:, dr, h, :], in_=t_kbf[h]), reads=[nm("t_kbf", h)], writes=[KB_])
                    P.op("dve", lambda e, h=h, tj=tj, eb3=eb3, col=col: e.tensor_tensor(out=t_kdT[tj].rearrange("p (c t) -> p c t", t=64), in0=t_kbf[h].rearrange("p (c t) -> p c t", t=64),
                                                                         in1=eb3[:, :, col:col + 1].to_broadcast([128, 4, 64]), op=ALU.mult),
                         reads=[nm("t_kbf", h), nm("t_eb", h)], writes=["t_kdT%d" % tj])
                    P.op("pool", lambda e, h=h, dr=dr, eb3=eb3, col=col: e.tensor_copy(out=ebl_st[bi][:, dr, h, :], in_=eb3[:, :, col]), reads=[nm("t_eb", h)], writes=[EB_])

                    def kd_tail(kdT_=t_kdT[tj], dr=dr, h=h, nm_="t_kdT%d" % tj):
                        pk, pkn = psK.next()
                        for s in range(2):
                            P.op("pe", lambda e, s=s: e.transpose(out=pk[:, s, :], in_=kdT_[:, s * 128:(s + 1) * 128], identity=ident),
                                 reads=[nm_, "ident"], writes=[pkn])
                        P.op("act", lambda e: e.copy(out=kd_st[bi][:, :, dr, h * 128:(h + 1) * 128], in_=pk), reads=[pkn], writes=[KD_])
                    kdp.append(kd_tail)
            P.dma("sp", lambda e, bi=bi, t0=t0: e.dma_start(out=VA[PAD + t0:PAD + t0 + TS, :].rearrange("(s p) f -> p s f", p=128),
                                                            in_=va_st[bi].rearrange("p s h d -> p s (h d)")), reads=[VA_], writes=["VA"], chan=VA_)
            for s_ in range(2):
                P.dma("sp", lambda e, bi=bi, s_=s_: e.dma_start(out=HV[:, :, 2 * stI + s_, :].rearrange("h p k -> p h k"), in_=hv_st[bi][:, s_, :].rearrange("p (h k) -> p h k", h=4)),
                      reads=[HV_], writes=["HV"], chan=HV_ + str(s_))
            for s_ in range(2):
                P.dma("sp", lambda e, bi=bi, s_=s_: e.dma_start(out=GS[:, :, 2 * stI + s_, :].rearrange("h p k -> p h k"), in_=gs_st[bi][:, s_, :].rearrange("p (h k) -> p h k", h=4)),
                      reads=[GS_], writes=["GS"], chan=GS_ + str(s_))
            def late(stI=stI, bi=bi, t0=t0, QB_=QB_, KB_=KB_, KD_=KD_, EB_=EB_):
                while kdp:
                    kdp.pop(0)()
                for dr in range(2):
                    P.dma("sp", lambda e, dr=dr, bi=bi, t0=t0: e.dma_start(out=QB[dr, :, :, t0:t0 + TS].rearrange("h p t -> p h t"), in_=qb_st[bi][:, dr, :, :]),
                          reads=[QB_], writes=["QB"], chan=QB_ + str(dr))
                    P.dma("sp", lambda e, dr=dr, bi=bi, t0=t0: e.dma_start(out=KB[dr, :, :, t0:t0 + TS].rearrange("h p t -> p h t"), in_=kb_st[bi][:, dr, :, :]),
                          reads=[KB_], writes=["KB"], chan=KB_ + str(dr))
                    for s_ in range(2):
                        P.dma("sp", lambda e, dr=dr, bi=bi, s_=s_: e.dma_start(out=KD[dr, :, :, 2 * stI + s_, :].rearrange("h p k -> p h k"), in_=kd_st[bi][:, s_, dr, :].rearrange("p (h k) -> p h k", h=4)),
                              reads=[KD_], writes=["KD"], chan=KD_ + str(dr) + str(s_))
                    P.dma("sp", lambda e, dr=dr, bi=bi, stI=stI: e.dma_start(out=EBL[dr, :, :, stI * 4:(stI + 1) * 4].rearrange("h p c -> p h c"), in_=ebl_st[bi][:, dr, :, :]),
                          reads=[EB_], writes=["EBL"], chan=EB_ + str(dr))
            late_q.append(late)


        load_x(0)
        if NST > 1:
            load_x(1)
        prep_tile(0)
        prep_tile_b(0)
        for stI in range(NST):
            do_tile(stI)
        while late_q:
            late_q.pop(0)()

        P.barrier()
        if "DBGA" in dbg:
            for nm, ap_, shp, dt_ in (("d_xt", xt[1], [128, 2 * D], F32), ("d_ss", ss[1], [128, 2], F32), ("d_xn", xn, [128, 2 * D], BF16),
                                      ("d_hT", hT[1], [128, 8 * TS], BF16), ("d_wn", wn, [128, D], F32), ("d_win", WIN[:, 0, :], [128, 4096], BF16)):
                dd = nc.dram_tensor(nm, shp, dt_, kind="ExternalOutput").ap()
                src = ap_
                if len(ap_.shape) == 3:
                    src = ap_.rearrange("p a b -> p (a b)")
                P.dma("sp", lambda e, dd=dd, src=src: e.dma_start(out=dd, in_=src), writes=[nm], chan=nm)
            P.barrier()
        def finish():
            for j in range(4):
                P.dma("sp", lambda e, j=j: e.dma_start(out=out[j * 128:(j + 1) * 128, 0:512], in_=zeros[:, 0:1024].bitcast(F32)), reads=["zeros"], writes=["out"], chan="fin%d" % j)
            P.barrier()
            P.emit(st)

        if stop_after == "A":
            finish()
            return nc, P

        A.off = const_end
        Eb = A.alloc([128, 24, 256], BF16)
        sel = A.alloc([128, 64], F32)
        Qs = A.alloc([128, 4, 2048], BF16)
        Ks = A.alloc([128, 4, 4096], BF16)
        V1 = A.alloc([128, 17, 520], BF16)
        V4 = A.alloc([128, 4, 5, 520], BF16)
        V16 = A.alloc([128, 16, 2, 520], BF16)
        acc = A.alloc([128, 4, 2048], F32)
        att = [A.alloc([128, 2048], BF16) for _ in range(2)]
        NPT = 12
        Pt = [A.alloc([128, 256], BF16) for _ in range(NPT)]
        estg = A.alloc([128, 2048], F32)
        rden = A.alloc([128, 512], F32)
        for g in range(3):
            P.dma("sp", lambda e, g=g: e.dma_start(out=estg.rearrange("p (a b) -> p a b", a=8), in_=emask[:, g * 8:(g + 1) * 8, :]), writes=["estg"], chan="estg")
            P.op("dve", lambda e, g=g: e.tensor_copy(out=Eb[:, g * 8:(g + 1) * 8, :], in_=estg.rearrange("p (a b) -> p a b", a=8)), reads=["estg"], writes=["Eb"])
        P.op("pool", lambda e: e.memset(sel, 0.0), writes=["sel"])
        P.op("pool", lambda e: e.memset(sel[64:65, :], 1.0), reads=["sel"], writes=["sel"])
        psS = Rot([(psv(i, [128, 256]), "bank%d" % i) for i in range(4)])
        psO = Rot([(psv(4 + i, [128, 128]), "bank%d" % (4 + i)) for i in range(3)])
        psB = Rot([(psv(7, [128, 512]), "bank7")])
        pt_i = [0]
        att_i = [0]
        bpend = []

        def attn_unit2(h0, hl0, pat, r, kbase, qbase, vt0, vt1, first, vtok):
            c = h0 // 2
            pss = [psS.next() for _ in range(2)]
            for kt in range(2):
                k0 = kbase + 128 * r * kt
                for hh_ in range(2):
                    pb = 64 * hh_
                    ps, psn = pss[hh_]
                    P.op("pe", lambda e, kt=kt, k0=k0, pb=pb, ps=ps: e.matmul(ps[:, kt * 128:(kt + 1) * 128], lhsT=Ks[pb:pb + 64, c, k0:k0 + 127 * r + 1:r],
                                                                        rhs=Qs[pb:pb + 64, c, qbase:qbase + 127 * r + 1:r], start=True, stop=True),
                         reads=["Ks", "Qs"], writes=[psn])
            for hh_ in range(2):
                h, hl = h0 + hh_, hl0 + hh_
                ps, psn = pss[hh_]
                ti = pt_i[0] % NPT
                pt_i[0] += 1
                PT_ = "Pt%d" % ti
                P.op("act", lambda e, ti=ti, ps=ps: e.activation(out=Pt[ti], in_=ps, func=AF.Exp), reads=[psn], writes=[PT_])
                P.op("pool", lambda e, ti=ti, h=h: e.tensor_tensor(out=Pt[ti], in0=Pt[ti], in1=Eb[:, h * 3 + pat, :], op=ALU.mult), reads=[PT_, "Eb"], writes=[PT_])

                def pv_tail(h=h, hl=hl, ti=ti, PT_=PT_):
                    po, pon = psO.next()
                    for kt, vt in enumerate((vt0, vt1)):
                        P.op("pe", lambda e, kt=kt, vt=vt: e.matmul(po[0:65, :], lhsT=vt[:, h * 65:(h + 1) * 65], rhs=Pt[ti][:, kt * 128:(kt + 1) * 128],
                                                                  start=(kt == 0), stop=(kt == 1)), reads=[PT_, vtok], writes=[pon])
                    dst = acc[0:65, hl, qbase:qbase + 127 * r + 1:r]
                    if first:
                        P.op("dve", lambda e: e.tensor_copy(out=dst, in_=po[0:65, :]), reads=[pon], writes=["acc%d" % hl])
                    else:
                        P.op("dve", lambda e: e.tensor_tensor(out=dst, in0=dst, in1=po[0:65, :], op=ALU.add), reads=[pon, "acc%d" % hl], writes=["acc%d" % hl])
                bpend.append(pv_tail)
            while len(bpend) > 10:
                bpend.pop(0)()

        def attn_block(jb):
            p0 = 2048 * jb
            P.dma("sp", lambda e: e.dma_start(out=Qs, in_=QT[:, :, p0:p0 + 2048].rearrange("c p t -> p c t")), writes=["Qs"], chan="Qs")
            P.dma("sp", lambda e: e.dma_start(out=Ks, in_=KT[:, :, p0:p0 + 4096].rearrange("c p t -> p c t")), writes=["Ks"], chan="Ks")
            r0 = PAD + p0 - 64
            P.dma("sp", lambda e: e.dma_start(out=V1, in_=VA[r0:r0 + 17 * 128, :].rearrange("(m p) f -> p m f", p=128)), writes=["V1"], chan="V1")
            for i in range(4):
                r4 = PAD + p0 + i - 256
                P.dma("sp", lambda e, i=i, r4=r4: e.dma_start(out=V4[:, i, :, :], in_=VA[r4:r4 + 639 * 4 + 1:4, :].rearrange("(m p) f -> p m f", p=128)),
                      writes=["V4_%d" % i], chan="V4_%d" % i)
            for i in range(16):
                r16 = PAD + p0 + i - 1024
                P.dma("sp", lambda e, i=i, r16=r16: e.dma_start(out=V16[:, i, :, :], in_=VA[r16:r16 + 255 * 16 + 1:16, :].rearrange("(m p) f -> p m f", p=128)),
                      writes=["V16_%d" % i], chan="V16_%d" % i)
            for hg in range(2):
              for hl0 in (0, 2):
                h0 = hg * 4 + hl0
                for j in range(16):
                    attn_unit2(h0, hl0, 0, 1, 1024 - 64 + 128 * j, 128 * j, V1[:, j, :], V1[:, j + 1, :], True, "V1")
                for i in range(4):
                    for j in range(4):
                        attn_unit2(h0, hl0, 1, 4, 1024 + i - 256 + 512 * j, i + 512 * j, V4[:, i, j, :], V4[:, i, j + 1, :], False, "V4_%d" % i)
                for i in range(16):
                    attn_unit2(h0, hl0, 2, 16, i, i, V16[:, i, 0, :], V16[:, i, 1, :], False, "V16_%d" % i)
                while bpend:
                    bpend.pop(0)()
                for hl in (hl0, hl0 + 1):
                    h = hg * 4 + hl
                    ai = att_i[0] % 2
                    att_i[0] += 1
                    AT_ = "att%d" % ai
                    for q4 in range(4):
                        pbk, pbn = psB.next()
                        P.op("pe", lambda e, pbk=pbk, q4=q4, hl=hl: e.matmul(pbk[0:64, :], lhsT=sel[0:65, :], rhs=acc[0:65, hl, q4 * 512:(q4 + 1) * 512], start=True, stop=True),
                             reads=["sel", "acc%d" % hl], writes=[pbn])
                        P.op("act", lambda e, pbk=pbk: e.activation(out=rden[0:64, :], in_=pbk[0:64, :], func=AF.Ln), reads=[pbn], writes=["rden"])
                        P.op("act", lambda e: e.activation(out=rden[0:64, :], in_=rden[0:64, :], func=AF.Exp, scale=-1.0), reads=["rden"], writes=["rden"])
                        P.op("dve", lambda e, q4=q4, hl=hl, ai=ai: e.tensor_tensor(out=att[ai][0:64, q4 * 512:(q4 + 1) * 512], in0=acc[0:64, hl, q4 * 512:(q4 + 1) * 512],
                                                                             in1=rden[0:64, :], op=ALU.mult), reads=["rden", "acc%d" % hl], writes=[AT_])
                    c, pb = h // 2, 64 * (h % 2)
                    P.dma("sp", lambda e, c=c, pb=pb, ai=ai: e.dma_start(out=MXT[c, pb:pb + 64, p0:p0 + 2048], in_=att[ai][0:64, :]), reads=[AT_], writes=["MXT"], chan=AT_)

        nblk = 4 if nst is None else max(1, (nst * TS) // 2048)
        for jb in range(nblk):
            if "B" not in skip:
                attn_block(jb)
        P.barrier()
        if stop_after == "B":
            finish()
            return nc, P

        A.off = const_end
        hm = A.alloc([128, 2, 128], F32)
        onwt = A.alloc([128, 128], F32)
        P.dma("sp", lambda e: e.dma_start(out=hm, in_=hmask), writes=["hm"], chan="hm")
        P.dma("sp", lambda e: e.dma_start(out=onwt, in_=onw.partition_broadcast(128)), writes=["onwt"], chan="onwt")
        qbT = [A.alloc([128, S], BF16) for _ in range(2)]
        kbT = [A.alloc([128, S], BF16) for _ in range(2)]
        kdt = [A.alloc([128, 64, 128], BF16) for _ in range(2)]
        vt_ = A.alloc([128, 64, 128], BF16)
        gst = A.alloc([128, 64, 128], BF16)
        eblt = A.alloc([128, 2, 128], F32)
        oacc = A.alloc([128, 64, 128], F32)
        Sf = [[A.alloc([128, 128], F32) for _ in range(2)] for _ in range(2)]
        Sb = [[A.alloc([128, 128], BF16) for _ in range(2)] for _ in range(2)]
        sc = [0, 0]
        Am = [A.alloc([128, 128], BF16) for _ in range(2)]
        ssq = A.alloc([128, 64], F32)
        junk2 = A.alloc([128, 128], BF16)
        bo = [A.alloc([128, 128], F32) for _ in range(2)]
        bo16 = [A.alloc([128, 128], BF16) for _ in range(2)]
        mstage = A.alloc([128, S], BF16)
        nT = 64 if nst is None else (nst * TS) // 128

        gst2 = [gst, A.alloc([128, 64, 128], BF16)]

        def head_loads(h):
            for d in range(2):
                P.dma("sp", lambda e, d=d: e.dma_start(out=qbT[d][:, 0:nT * 128], in_=QB[d, h, :, 0:nT * 128]), writes=["qbT%d" % d], chan="qbT%d" % d)
                P.dma("sp", lambda e, d=d: e.dma_start(out=kbT[d][:, 0:nT * 128], in_=KB[d, h, :, 0:nT * 128]), writes=["kbT%d" % d], chan="kbT%d" % d)
                P.dma("sp", lambda e, d=d: e.dma_start(out=kdt[d][:, 0:nT, :], in_=KD[d, h, :, 0:nT, :]),
                      writes=["kdt%d" % d], chan="kdt%d" % d)
            P.dma("sp", lambda e: e.dma_start(out=vt_[:, 0:nT, :], in_=HV[h, :, 0:nT, :]), writes=["vt"], chan="vt")
            P.dma("sp", lambda e: e.dma_start(out=gst2[h % 2][:, 0:nT, :], in_=GS[h, :, 0:nT, :]), writes=["gst%d" % (h % 2)], chan="gst%d" % (h % 2))
            P.dma("sp", lambda e: e.dma_start(out=eblt[:, :, 0:2 * nT], in_=EBL[:, h, :, 0:2 * nT].rearrange("d p c -> p d c")), writes=["eblt"], chan="eblt")

        def hgrn_head(h):
            gst = gst2[h % 2]
            GST_ = "gst%d" % (h % 2)
            for d in range(2):
                sc[d] = 0
                P.op("pool", lambda e, d=d: e.memset(Sf[d][0], 0.0), writes=["Sf%d_0" % d])
                P.op("pool", lambda e, d=d: e.memset(Sb[d][0], 0.0), writes=["Sb%d_0" % d])

            def step(stp):
                Ts = (stp, nT - 1 - stp)
                halves = ((0, 1), (1, 0))
                pA = [psv(d, [128, 128]) for d in range(2)]
                pU = [[psv(2 + 2 * d + i, [128, 128]) for i in range(2)] for d in range(2)]
                pO = [psv(6 + d, [128, 128]) for d in range(2)]
                for d in range(2):
                    T = Ts[d]
                    P.op("pe", lambda e, d=d, T=T: e.matmul(pA[d], lhsT=kbT[d][:, T * 128:(T + 1) * 128], rhs=qbT[d][:, T * 128:(T + 1) * 128], start=True, stop=True),
                         reads=["kbT%d" % d, "qbT%d" % d], writes=["bank%d" % d])
                for d in range(2):
                    T = Ts[d]
                    for i, hc in enumerate(halves[d]):
                        P.op("pe", lambda e, d=d, T=T, i=i, hc=hc: e.matmul(pU[d][i], lhsT=kdt[d][hc * 64:(hc + 1) * 64, T, :], rhs=vt_[hc * 64:(hc + 1) * 64, T, :], start=True, stop=True),
                             reads=["kdt%d" % d, "vt"], writes=["bank%d" % (2 + 2 * d + i)])
                for d in range(2):
                    P.op("dve", lambda e, d=d: e.tensor_tensor(out=Am[d], in0=pA[d], in1=hm[:, d, :], op=ALU.mult), reads=["bank%d" % d, "hm"], writes=["Am%d" % d])
                for i in range(2):
                    for d in range(2):
                        T = Ts[d]
                        hc = halves[d][i]
                        P.op("pe", lambda e, d=d, T=T, hc=hc: e.matmul(pO[d][hc * 64:(hc + 1) * 64, :], lhsT=Am[d][hc * 64:(hc + 1) * 64, hc * 64:(hc + 1) * 64],
                                                                     rhs=vt_[hc * 64:(hc + 1) * 64, T, :], start=True, stop=False),
                             reads=["Am%d" % d, "vt"], writes=["bank%d" % (6 + d)])
                        c0 = T * 128 + hc * 64
                        cu, nx = sc[d] % 2, (sc[d] + 1) % 2
                        sc[d] += 1
                        P.op("pe", lambda e, d=d, hc=hc, c0=c0, i=i, cu=cu: e.matmul(pO[d][hc * 64:(hc + 1) * 64, :], lhsT=qbT[d][:, c0:c0 + 64], rhs=Sb[d][cu], start=False, stop=True),
                             reads=["qbT%d" % d, "Sb%d_%d" % (d, cu)], writes=["bank%d" % (6 + d)])
                        ch = 2 * T + hc
                        P.op("dve", lambda e, d=d, i=i, ch=ch, cu=cu, nx=nx: e.scalar_tensor_tensor(out=Sf[d][nx], in0=Sf[d][cu], scalar=eblt[:, d, ch:ch + 1], in1=pU[d][i], op0=ALU.mult, op1=ALU.add),
                             reads=["Sf%d_%d" % (d, cu), "eblt", "bank%d" % (2 + 2 * d + i)], writes=["Sf%d_%d" % (d, nx)])
                        P.op("act", lambda e, d=d, nx=nx: e.copy(out=Sb[d][nx], in_=Sf[d][nx]), reads=["Sf%d_%d" % (d, nx)], writes=["Sb%d_%d" % (d, nx)])
                for d in range(2):
                    T = Ts[d]
                    if stp < nT // 2:
                        P.op("act", lambda e, d=d, T=T: e.copy(out=oacc[:, T, :], in_=pO[d]), reads=["bank%d" % (6 + d)], writes=["oacc%d" % T])
                    else:
                        P.op("dve", lambda e, d=d, T=T: e.tensor_tensor(out=oacc[:, T, :], in0=oacc[:, T, :], in1=pO[d], op=ALU.add),
                             reads=["bank%d" % (6 + d), "oacc%d" % T], writes=["oacc%d" % T])

            for stp in range(nT):
                step(stp)
            if h + 1 < 4:
                head_loads(h + 1)
            for T in range(nT):
                P.op("act", lambda e, T=T: e.activation(out=junk2, in_=oacc[:, T, :], func=AF.Square, accum_out=ssq[:, T:T + 1]), reads=["oacc%d" % T], writes=["junk2", "ssq"])
            P.op("act", lambda e: e.activation(out=ssq[:, 0:nT], in_=ssq[:, 0:nT], func=AF.Ln, scale=1.0 / 128, bias=EPS), reads=["ssq"], writes=["ssq"])
            P.op("act", lambda e: e.activation(out=ssq[:, 0:nT], in_=ssq[:, 0:nT], func=AF.Exp, scale=-0.5), reads=["ssq"], writes=["ssq"])
            for T in range(nT):
                i2 = T % 2
                P.op("dve", lambda e, T=T, i2=i2: e.scalar_tensor_tensor(out=bo[i2], in0=oacc[:, T, :], scalar=ssq[:, T:T + 1], in1=onwt, op0=ALU.mult, op1=ALU.mult),
                     reads=["oacc%d" % T, "ssq", "onwt"], writes=["bo%d" % i2])
                P.op("pool", lambda e, T=T, i2=i2: e.tensor_tensor(out=bo16[i2], in0=bo[i2], in1=gst[:, T, :], op=ALU.mult), reads=["bo%d" % i2, GST_], writes=["bo16_%d" % i2])
                pT = psv(i2, [128, 128], BF16)
                P.op("pe", lambda e, i2=i2, pT=pT: e.transpose(out=pT, in_=bo16[i2], identity=ident), reads=["bo16_%d" % i2, "ident"], writes=["bank%d" % i2])
                P.op("act", lambda e, T=T, pT=pT: e.copy(out=mstage[:, T * 128:(T + 1) * 128], in_=pT), reads=["bank%d" % i2], writes=["mstage"])
            P.dma("sp", lambda e: e.dma_start(out=MXT[4 + h, :, 0:nT * 128], in_=mstage[:, 0:nT * 128]), reads=["mstage"], writes=["MXT"], chan="mstage")

        head_loads(0)
        for h in range(4):
            hgrn_head(h)
        P.barrier()
        if stop_after == "C":
            finish()
            return nc, P

        A.off = const_end
        nL = nT // 2
        CAPK = float(nT * 128 // 8)
        AFF = A.alloc([128, 64, 16], F32)
        posm = A.alloc([128, 32, 16], F32)
        nposm = A.alloc([128, 32, 16], F32)
        RT = A.alloc([128, 32, 16, 5], BF16)
        IOT = A.alloc([128, 1024], F32)
        idx_all = A.alloc([128, 16, 8], mybir.dt.int32)
        gate_all = A.alloc([128, 16, 8], F32)
        dumt = A.alloc([128, 2], F32)
        ones_b = A.alloc([128, 128], F32)
        P.op("pool", lambda e: e.memset(ones_b, 1.0), writes=["ones_b"])
        persist_end = A.off
        persistF_end = A.off
        WO = A.alloc([128, 8, 1024], BF16)
        WR = A.alloc([128, 8, 16], BF16)
        wrs = A.alloc([128, 8, 16], F32)
        n2t = A.alloc([128, D], F32)
        wst = [A.alloc([128, 2048], F32) for _ in range(2)]
        xt2 = [A.alloc([128, D], F32) for _ in range(2)]
        mx = [A.alloc([128, 8, 128], BF16) for _ in range(2)]
        x1t = [A.alloc([128, D], F32) for _ in range(2)]
        h2 = [A.alloc([128, D], BF16) for _ in range(2)]
        h2T = [A.alloc([128, 8, 128], BF16) for _ in range(2)]
        junk3 = A.alloc([128, D], BF16)
        ss2 = [A.alloc([128, 1], F32) for _ in range(2)]
        rmx = [A.alloc([128, 1], F32) for _ in range(2)]
        rsm = [A.alloc([128, 1], F32) for _ in range(2)]
        ex = [A.alloc([128, 16], F32) for _ in range(2)]
        P.dma("sp", lambda e: e.dma_start(out=n2t, in_=n2w.partition_broadcast(128)), writes=["n2t"], chan="n2t")
        P.dma("sp", lambda e: e.dma_start(out=wrs, in_=w_r.rearrange("(c p) e -> p c e", p=128)), writes=["wrs"], chan="wrs")
        P.op("dve", lambda e: e.tensor_copy(out=WR, in_=wrs), reads=["wrs"], writes=["WR"])
        for c2 in range(4):
            sg, sgn = wst[c2 % 2], "wst%d" % (c2 % 2)
            P.dma("sp", lambda e, c2=c2, sg=sg: e.dma_start(out=sg.rearrange("p (a b) -> p a b", a=2), in_=w_out[c2 * 256:(c2 + 1) * 256, :].rearrange("(a p) n -> p a n", p=128)),
                  writes=[sgn], chan=sgn)
            P.op("dve" if c2 % 2 else "pool", lambda e, c2=c2, sg=sg: e.tensor_copy(out=WO[:, 2 * c2:2 * c2 + 2, :], in_=sg.rearrange("p (a b) -> p a b", a=2)),
                 reads=[sgn], writes=["WO"])
        if "B" in skip:
            for c in range(4):
                for q in range(0, nT * 128, 2048):
                    w_ = min(2048, nT * 128 - q)
                    P.dma("sp", lambda e, c=c, q=q, w_=w_: e.dma_start(out=MXT[c, :, q:q + w_], in_=zeros[:, 0:w_]), reads=["zeros"], writes=["MXT"], chan="zmx")

        P.dma("sp", lambda e: e.dma_start(out=H2[S // 2:S // 2 + 128, :], in_=zeros[:, 0:1024]), reads=["zeros"], writes=["H2pad"], chan="h2pad")
        P.dma("sp", lambda e: e.dma_start(out=out[S // 2:S // 2 + 128, :], in_=zeros[:, 0:2048].bitcast(F32)), reads=["zeros"], writes=["outpad"], chan="outpad")

        if nst is not None:
            for r0 in range(nL * 128, S // 2, 128):
                P.dma("sp", lambda e, r0=r0: e.dma_start(out=H2[r0:r0 + 128, :], in_=zeros[:, 0:1024]), reads=["zeros"], writes=["H2pad"], chan="h2pad")
                P.dma("sp", lambda e, r0=r0: e.dma_start(out=out[r0:r0 + 128, :], in_=zeros[:, 0:2048].bitcast(F32)), reads=["zeros"], writes=["outpad"], chan="outpad")

        def d_load(T):
            bi = T % 2
            P.dma("sp", lambda e: e.dma_start(out=xt2[bi], in_=x[T * 128:(T + 1) * 128, :]), writes=["xt2_%d" % bi], chan="xt2_%d" % bi)
            P.dma("sp", lambda e: e.dma_start(out=mx[bi], in_=MXT[:, :, T * 128:(T + 1) * 128].rearrange("c p t -> p c t")), reads=["MXT"], writes=["mx%d" % bi], chan="mx%d" % bi)

        def d_tile(T):
            bi = T % 2
            X_, M_, X1_, H2_, HT_ = "xt2_%d" % bi, "mx%d" % bi, "x1t%d" % bi, "h2_%d" % bi, "h2T%d" % bi
            for half in range(2):
                pX = psv(2 * bi + half, [128, 512])
                for m in range(8):
                    P.op("pe", lambda e, half=half, m=m, pX=pX: e.matmul(pX, lhsT=mx[bi][:, m, :], rhs=WO[:, m, half * 512:(half + 1) * 512], start=(m == 0), stop=(m == 7)),
                         reads=[M_, "WO"], writes=["bank%d" % (2 * bi + half)])
                P.op("dve", lambda e, half=half, pX=pX: e.tensor_tensor(out=x1t[bi][:, half * 512:(half + 1) * 512], in0=xt2[bi][:, half * 512:(half + 1) * 512], in1=pX, op=ALU.add),
                     reads=[X_, "bank%d" % (2 * bi + half)], writes=[X1_])
            if T + 2 < nT:
                d_load(T + 2)
            if T < nL:
                P.dma("sp", lambda e: e.dma_start(out=out[T * 128:(T + 1) * 128, :], in_=x1t[bi]), reads=[X1_], writes=["out"], chan=X1_)
            S2_ = "ss2_%d" % bi
            P.op("act", lambda e: e.activation(out=junk3, in_=x1t[bi], func=AF.Square, accum_out=ss2[bi]), reads=[X1_], writes=["junk3", S2_])
            P.op("act", lambda e: e.activation(out=ss2[bi], in_=ss2[bi], func=AF.Ln, scale=1.0 / D, bias=EPS), reads=[S2_], writes=[S2_])
            P.op("act", lambda e: e.activation(out=ss2[bi], in_=ss2[bi], func=AF.Exp, scale=-0.5), reads=[S2_], writes=[S2_])
            P.op("dve", lambda e: e.scalar_tensor_tensor(out=h2[bi], in0=x1t[bi], scalar=ss2[bi][:, 0:1], in1=n2t, op0=ALU.mult, op1=ALU.mult),
                 reads=[X1_, S2_, "n2t"], writes=[H2_])
            if T < nL:
                P.dma("sp", lambda e: e.dma_start(out=H2[T * 128:(T + 1) * 128, :], in_=h2[bi]), reads=[H2_], writes=["H2"], chan=H2_)

        def d_tile2(T):
            bi = T % 2
            X_, M_, X1_, H2_, HT_ = "xt2_%d" % bi, "mx%d" % bi, "x1t%d" % bi, "h2_%d" % bi, "h2T%d" % bi
            pT = psv(4 + bi, [128, 8, 128], BF16)
            for c in range(8):
                P.op("pe", lambda e, c=c: e.transpose(out=pT[:, c, :], in_=h2[bi][:, c * 128:(c + 1) * 128], identity=ident), reads=[H2_, "ident"], writes=["bank%d" % (4 + bi)])
            P.op("act", lambda e: e.copy(out=h2T[bi], in_=pT), reads=["bank%d" % (4 + bi)], writes=[HT_])
            pR = psv(6 + bi, [128, 16])
            for c in range(8):
                P.op("pe", lambda e, c=c: e.matmul(pR, lhsT=h2T[bi][:, c, :], rhs=WR[:, c, :], start=(c == 0), stop=(c == 7)), reads=[HT_, "WR"], writes=["bank%d" % (6 + bi)])
            R_ = "rt%d" % bi
            P.op("dve", lambda e: e.tensor_reduce(out=rmx[bi], in_=pR, op=ALU.max, axis=AX.X), reads=["bank%d" % (6 + bi)], writes=[R_ + "m"])
            P.op("dve", lambda e: e.tensor_scalar(out=rmx[bi], in0=rmx[bi], scalar1=-1.0, scalar2=None, op0=ALU.mult), reads=[R_ + "m"], writes=[R_ + "m"])
            P.op("act", lambda e: e.activation(out=ex[bi], in_=pR, func=AF.Exp, bias=rmx[bi][:, 0:1], accum_out=rsm[bi]), reads=["bank%d" % (6 + bi), R_ + "m"], writes=[R_ + "e", R_ + "s"])
            P.op("dve", lambda e: e.reciprocal(out=rsm[bi], in_=rsm[bi]), reads=[R_ + "s"], writes=[R_ + "s"])
            P.op("dve", lambda e: e.tensor_scalar(out=AFF[:, T, :], in0=ex[bi], scalar1=rsm[bi][:, 0:1], scalar2=None, op0=ALU.mult), reads=[R_ + "e", R_ + "s"], writes=["AFF"])

        d_load(0)
        if nT > 1:
            d_load(1)
        d_tile(0)
        for T in range(nT):
            if T + 1 < nT:
                d_tile(T + 1)
            d_tile2(T)
        P.barrier()

        A.off = persist_end
        lo = A.alloc([128, 16], F32)
        hi = A.alloc([128, 16], F32)
        mid = A.alloc([128, 16], F32)
        d1 = A.alloc([128, 16], F32)
        ge = A.alloc([128, 16], F32)
        cmpb = A.alloc([128, 64, 16], BF16)
        cnt = A.alloc([128, 16], F32)
        mk = A.alloc([128, 32, 16], F32)
        P.op("pool", lambda e: e.memset(lo, 0.0), writes=["lo"])
        P.op("pool", lambda e: e.memset(hi, 1.0), writes=["hi"])
        pC = psv(0, [128, 16])
        for it in range(28):
            P.op("dve", lambda e: e.scalar_tensor_tensor(out=mid, in0=hi, scalar=0.5, in1=lo, op0=ALU.mult, op1=ALU.add), reads=["lo", "hi"], writes=["mid"])
            P.op("pool", lambda e: e.tensor_scalar(out=hi, in0=hi, scalar1=0.5, scalar2=0.0, op0=ALU.mult, op1=ALU.add), reads=["hi"], writes=["hi"])
            P.op("dve", lambda e: e.tensor_tensor(out=cmpb[:, 0:nT, :], in0=AFF[:, 0:nT, :], in1=mid.unsqueeze(1).to_broadcast([128, nT, 16]), op=ALU.is_gt),
                 reads=["AFF", "mid"], writes=["cmpb"])
            P.op("dve", lambda e: e.tensor_reduce(out=cnt, in_=cmpb[:, 0:nT, :].rearrange("p t e -> p e t"), op=ALU.add, axis=AX.X), reads=["cmpb"], writes=["cnt"])
            P.op("pe", lambda e: e.matmul(pC, lhsT=ones_b, rhs=cnt, start=True, stop=True), reads=["ones_b", "cnt"], writes=["bank0"])
            P.op("dve", lambda e: e.tensor_scalar(out=ge, in0=pC, scalar1=CAPK - 0.5, scalar2=None, op0=ALU.is_gt), reads=["bank0"], writes=["ge"])
            P.op("dve", lambda e: e.tensor_tensor(out=d1, in0=ge, in1=hi, op=ALU.mult), reads=["ge", "hi"], writes=["d1"])
            P.op("dve", lambda e: e.tensor_tensor(out=lo, in0=lo, in1=d1, op=ALU.add), reads=["lo", "d1"], writes=["lo"])
        P.op("dve", lambda e: e.tensor_tensor(out=mk[:, 0:nL, :], in0=AFF[:, 0:nL, :], in1=lo.unsqueeze(1).to_broadcast([128, nL, 16]), op=ALU.is_gt), reads=["AFF", "lo"], writes=["mk"])
        NE = nL * 16
        mkb = A.alloc([128, 32, 16], BF16)
        ustr = A.alloc([128, 128], BF16)
        ustf = A.alloc([128, 128], F32)
        onesb = A.alloc([128, 128], BF16)
        totE = A.alloc([128, 16, 32], F32)
        incE = A.alloc([128, 16, 32], F32)
        rmE = A.alloc([128, 16, 32], F32)
        pos = A.alloc([128, 32, 16], F32)
        tka = A.alloc([128, 32, 3], F32)
        ahi = A.alloc([128, 32, 16], BF16)
        ahf = A.alloc([128, 32, 16], F32)
        P.op("pool", lambda e: e.memset(ustf, 1.0), writes=["ustf"])
        P.op("pool", lambda e: e.affine_select(out=ustf, in_=ustf, pattern=[[1, 128]], compare_op=ALU.is_gt, fill=0.0, base=0, channel_multiplier=-1), reads=["ustf"], writes=["ustf"])
        P.op("dve", lambda e: e.tensor_copy(out=ustr, in_=ustf), reads=["ustf"], writes=["ustr"])
        P.op("pool", lambda e: e.memset(onesb, 1.0), writes=["onesb"])
        P.op("pool", lambda e: e.memset(rmE, 1.0), writes=["rmE"])
        P.op("pool", lambda e: e.memset(rmE[:, :, 0:1], 0.0), reads=["rmE"], writes=["rmE"])
        P.op("pool", lambda e: e.memset(totE, 0.0), writes=["totE"])
        P.dma("sp", lambda e: e.dma_start(out=tka, in_=tokab), writes=["tka"], chan="tka")
        P.dma("sp", lambda e: e.dma_start(out=IOT, in_=iot), writes=["IOT"], chan="IOT")
        P.dma("sp", lambda e: e.dma_start(out=dumt, in_=dum), writes=["dumt"], chan="dumt")
        P.op("dve", lambda e: e.tensor_copy(out=mkb[:, 0:nL, :], in_=mk[:, 0:nL, :]), reads=["mk"], writes=["mkb"])
        pW = psv(1, [128, 32, 16])
        pTt = psv(2, [128, 32, 16])
        P.op("pe", lambda e: e.matmul(pW[:, 0:nL, :], lhsT=ustr, rhs=mkb[:, 0:nL, :], start=True, stop=True), reads=["ustr", "mkb"], writes=["bank1"])
        P.op("pe", lambda e: e.matmul(pTt[:, 0:nL, :], lhsT=onesb, rhs=mkb[:, 0:nL, :], start=True, stop=True), reads=["onesb", "mkb"], writes=["bank2"])
        P.op("dve", lambda e: e.tensor_copy(out=totE[:, :, 0:nL].rearrange("p e t -> p t e"), in_=pTt[:, 0:nL, :]), reads=["bank2", "totE"], writes=["totE"])
        P.op("dve", lambda e: e.tensor_tensor_scan(out=incE.rearrange("p e t -> p (e t)"), data0=rmE.rearrange("p e t -> p (e t)"), data1=totE.rearrange("p e t -> p (e t)"),
                                                   initial=0.0, op0=ALU.mult, op1=ALU.add), reads=["rmE", "totE"], writes=["incE"])
        P.op("dve", lambda e: e.tensor_tensor(out=incE, in0=incE, in1=totE, op=ALU.subtract), reads=["incE", "totE"], writes=["incE"])
        P.op("dve", lambda e: e.tensor_tensor(out=pos[:, 0:nL, :], in0=pW[:, 0:nL, :], in1=incE[:, :, 0:nL].rearrange("p e t -> p t e"), op=ALU.add), reads=["bank1", "incE"], writes=["pos"])
        P.op("dve", lambda e: e.scalar_tensor_tensor(out=posm[:, 0:nL, :], in0=pos[:, 0:nL, :], scalar=1.0, in1=mk[:, 0:nL, :], op0=ALU.add, op1=ALU.mult), reads=["pos", "mk"], writes=["posm"])
        P.op("dve", lambda e: e.tensor_scalar(out=nposm[:, 0:nL, :], in0=posm[:, 0:nL, :], scalar1=-1.0, scalar2=1.0, op0=ALU.mult, op1=ALU.add), reads=["posm"], writes=["nposm"])
        for k3 in range(3):
            P.op("dve", lambda e, k3=k3: e.tensor_copy(out=RT[:, 0:nL, :, k3], in_=tka[:, 0:nL, k3:k3 + 1].to_broadcast([128, nL, 16])), reads=["tka", "RT"], writes=["RT"])
        P.op("dve", lambda e: e.tensor_copy(out=ahi[:, 0:nL, :], in_=AFF[:, 0:nL, :]), reads=["AFF"], writes=["ahi"])
        P.op("dve", lambda e: e.tensor_copy(out=RT[:, 0:nL, :, 3], in_=ahi[:, 0:nL, :]), reads=["ahi", "RT"], writes=["RT"])
        P.op("dve", lambda e: e.tensor_copy(out=ahf[:, 0:nL, :], in_=ahi[:, 0:nL, :]), reads=["ahi"], writes=["ahf"])
        P.op("dve", lambda e: e.tensor_tensor(out=ahf[:, 0:nL, :], in0=AFF[:, 0:nL, :], in1=ahf[:, 0:nL, :], op=ALU.subtract), reads=["AFF", "ahf"], writes=["ahf"])
        P.op("dve", lambda e: e.tensor_copy(out=RT[:, 0:nL, :, 4], in_=ahf[:, 0:nL, :]), reads=["ahf", "RT"], writes=["RT"])
        CAPS = min(1024, nL * 128)
        NJ = CAPS // 128
        SW = min(512, CAPS)
        NSS = CAPS // SW
        selt = [A.alloc([128, 1024], BF16) for _ in range(4)]
        sela = [A.alloc([128, 1024], F32) for _ in range(3)]
        idxT = [A.alloc([128, 1024], F32) for _ in range(2)]
        tab = A.alloc([128, 8, 5], F32)
        t1 = A.alloc([128, 8], F32)
        t2 = A.alloc([128, 8], F32)
        csum = A.alloc([128, 16], F32)
        pCs = psv(5, [128, 16])
        for ex_ in range(16):
            for T in range(nL):
                P.op("pe", lambda e, ex_=ex_, T=T: e.matmul(pCs[0:5, ex_:ex_ + 1], lhsT=RT[:, T, ex_, :], rhs=onesb[:, 0:1], start=(T == 0), stop=(T == nL - 1)),
                     reads=["RT", "onesb"], writes=["bank5"])
        P.op("dve", lambda e: e.tensor_copy(out=csum[0:5, :], in_=pCs[0:5, :]), reads=["bank5"], writes=["csum"])
        sel_i = [0]

        def e2_expert(ex_):
            pb_ = 2 * (ex_ % 2)
            pI = [psv(pb_ + hf, [128, SW]) for hf in range(NSS)]
            for T in range(nL):
                si = sel_i[0] % 4
                sa = sel_i[0] % 3
                sel_i[0] += 1
                SL_, SA_ = "selt%d" % si, "sela%d" % sa
                P.op("act", lambda e, T=T, sa=sa: e.activation(out=sela[sa][:, 0:CAPS], in_=IOT[:, 0:CAPS], func=AF.Square, bias=nposm[:, T, ex_:ex_ + 1], scale=1.0),
                     reads=["IOT", "nposm"], writes=[SA_])
                P.op("dve", lambda e, si=si, sa=sa: e.tensor_scalar(out=selt[si][:, 0:CAPS], in0=sela[sa][:, 0:CAPS], scalar1=1.0, scalar2=-1.0, op0=ALU.min, op1=ALU.mult),
                     reads=[SA_], writes=[SL_])
                for hf in range(NSS):
                    P.op("pe", lambda e, T=T, si=si, hf=hf: e.matmul(pI[hf][0:5, :], lhsT=RT[:, T, ex_, :], rhs=selt[si][:, hf * SW:(hf + 1) * SW], start=(T == 0), stop=(T == nL - 1)),
                         reads=["RT", SL_], writes=["bank%d" % (pb_ + hf)])
            it = ex_ % 2
            IT_ = "idxT%d" % it
            for hf in range(NSS):
                P.op("act", lambda e, hf=hf: e.activation(out=idxT[it][0:5, hf * SW:(hf + 1) * SW], in_=pI[hf][0:5, :], func=AF.Identity, bias=csum[0:5, ex_:ex_ + 1], scale=1.0),
                     reads=["bank%d" % (pb_ + hf), "csum"], writes=[IT_])
            pJ = psv(4, [128, 8, 5])
            for j in range(NJ):
                P.op("pe", lambda e, j=j: e.transpose(out=pJ[:, j, :], in_=idxT[it][0:5, j * 128:(j + 1) * 128], identity=identf[0:5, 0:5]), reads=[IT_, "identf"], writes=["bank4"])
            P.op("dve", lambda e: e.tensor_copy(out=tab[:, 0:NJ, :], in_=pJ[:, 0:NJ, :]), reads=["bank4"], writes=["tab"])
            P.op("dve", lambda e: e.scalar_tensor_tensor(out=t1[:, 0:NJ], in0=tab[:, 0:NJ, 0], scalar=64.0, in1=tab[:, 0:NJ, 1], op0=ALU.mult, op1=ALU.add), reads=["tab"], writes=["t1"])
            P.op("dve", lambda e: e.tensor_scalar(out=t2[:, 0:NJ], in0=tab[:, 0:NJ, 2], scalar1=dumt[:, 1:2], scalar2=dumt[:, 0:1], op0=ALU.mult, op1=ALU.add), reads=["tab", "dumt"], writes=["t2"])
            P.op("dve", lambda e: e.tensor_tensor(out=t1[:, 0:NJ], in0=t1[:, 0:NJ], in1=t2[:, 0:NJ], op=ALU.add), reads=["t1", "t2"], writes=["t1"])
            P.op("dve", lambda e: e.tensor_copy(out=idx_all[:, ex_, 0:NJ], in_=t1[:, 0:NJ]), reads=["t1"], writes=["idx_all"])
            P.op("dve", lambda e: e.tensor_tensor(out=gate_all[:, ex_, 0:NJ], in0=tab[:, 0:NJ, 3], in1=tab[:, 0:NJ, 4], op=ALU.add), reads=["tab"], writes=["gate_all"])

        for ex_ in range(16):
            e2_expert(ex_)
        P.barrier()

        A.off = persistF_end
        Wg_b = [A.alloc([128, 8, 1024], BF16) for _ in range(2)]
        Wu_b = [A.alloc([128, 8, 1024], BF16) for _ in range(2)]
        Wd_b = [A.alloc([128, 8, 1024], BF16) for _ in range(2)]
        wst2 = [A.alloc([128, 2048], F32) for _ in range(3)]
        xin = [A.alloc([128, D], BF16) for _ in range(8)]
        xinT = [A.alloc([128, 8, 512], BF16) for _ in range(2)]
        hid = A.alloc([128, 8, 512], BF16)
        s_g = [A.alloc([128, 512], F32) for _ in range(2)]
        y32 = [A.alloc([128, D], F32) for _ in range(3)]
        wl_i = [0]
        xin_i = [0]
        y_i = [0]
        cast_engs = ("act", "dve", "act")

        def bg_items(ex_):
            wb = ex_ % 2
            ii = ex_ % 2
            items = []

            def w_chunk(src, dstl, nm, c2):
                def f_():
                    k = wl_i[0]
                    wl_i[0] += 1
                    sg, sgn = wst2[k % 3], "wst2_%d" % (k % 3)
                    P.dma("sp", lambda e: e.dma_start(out=sg.rearrange("p (a b) -> p a b", a=2), in_=src[ex_, c2 * 256:(c2 + 1) * 256, :].rearrange("(a p) n -> p a n", p=128)),
                          writes=[sgn], chan=sgn)
                    eng = cast_engs[k % 3]
                    if eng == "act":
                        P.op("act", lambda e: e.copy(out=dstl[wb][:, 2 * c2:2 * c2 + 2, :], in_=sg.rearrange("p (a b) -> p a b", a=2)), reads=[sgn], writes=[nm])
                    else:
                        P.op(eng, lambda e: e.tensor_copy(out=dstl[wb][:, 2 * c2:2 * c2 + 2, :], in_=sg.rearrange("p (a b) -> p a b", a=2)), reads=[sgn], writes=[nm])
                return f_

            for src, dstl, nm in ((w_g, Wg_b, "Wg%d" % wb), (w_u, Wu_b, "Wu%d" % wb), (w_d, Wd_b, "Wd%d" % wb)):
                for c2 in range(4):
                    items.append(w_chunk(src, dstl, nm, c2))
            return items

        def gather_ss(g):
            ex_, ssi = g // NSS, g % NSS
            for sub in range(SW // 128):
                j = ssi * (SW // 128) + sub
                xi = (g % 2) * 4 + sub
                XI_ = "xin%d" % xi
                P.dma("pool", lambda e, j=j, xi=xi: e.indirect_dma_start(out=xin[xi][:, :], out_offset=None, in_=H2[:, :],
                                                                     in_offset=bass.IndirectOffsetOnAxis(ap=idx_all[:, ex_, j:j + 1], axis=0)),
                      reads=["H2", "idx_all"], writes=[XI_], chan=XI_)

        def transp_ss(g):
            for sub in range(SW // 128):
                xi = (g % 2) * 4 + sub
                XI_, XT_ = "xin%d" % xi, "xinT%d" % (g % 2)
                pX = psv(2, [128, 8, 128], BF16)
                for c in range(8):
                    P.op("pe", lambda e, c=c, xi=xi, pX=pX: e.transpose(out=pX[:, c, :], in_=xin[xi][:, c * 128:(c + 1) * 128], identity=ident), reads=[XI_, "ident"], writes=["bank2"])
                P.op("act", lambda e, sub=sub, pX=pX: e.copy(out=xinT[g % 2][:, :, sub * 128:(sub + 1) * 128], in_=pX), reads=["bank2"], writes=[XT_])

        NG = 16 * NSS

        def moe_ss(g, bg):
            ex_, ssi = g // NSS, g % NSS
            wb = ex_ % 2
            G_, U_, D_ = "Wg%d" % wb, "Wu%d" % wb, "Wd%d" % wb
            XT_ = "xinT%d" % (g % 2)
            xT = xinT[g % 2]
            nsub = SW // 128

            def pop(n_):
                for _ in range(n_):
                    if bg:
                        bg.pop(0)()

            if g + 1 < NG:
                gather_ss(g + 1)
            for f in range(8):
                pi = f % 2
                pG, pU = psv(3 + pi, [128, SW]), psv(5 + pi, [128, SW])
                for c in range(8):
                    P.op("pe", lambda e, c=c, f=f, pG=pG: e.matmul(pG, lhsT=Wg_b[wb][:, c, f * 128:(f + 1) * 128], rhs=xT[:, c, 0:SW], start=(c == 0), stop=(c == 7)),
                         reads=[G_, XT_], writes=["bank%d" % (3 + pi)])
                for c in range(8):
                    P.op("pe", lambda e, c=c, f=f, pU=pU: e.matmul(pU, lhsT=Wu_b[wb][:, c, f * 128:(f + 1) * 128], rhs=xT[:, c, 0:SW], start=(c == 0), stop=(c == 7)),
                         reads=[U_, XT_], writes=["bank%d" % (5 + pi)])
                S_ = "s_g%d" % pi
                P.op("act", lambda e, pi=pi, pG=pG: e.activation(out=s_g[pi][:, 0:SW], in_=pG, func=AF.Silu), reads=["bank%d" % (3 + pi)], writes=[S_])
                P.op("dve", lambda e, pi=pi, pU=pU, f=f: e.tensor_tensor(out=hid[:, f, 0:SW], in0=pU, in1=s_g[pi][:, 0:SW], op=ALU.mult), reads=["bank%d" % (5 + pi), S_], writes=["hid"])
                pop(1)
            if g + 1 < NG:
                transp_ss(g + 1)
            for sub in range(nsub):
                j = ssi * nsub + sub
                yi = y_i[0] % 3
                y_i[0] += 1
                Y_ = "y32_%d" % yi
                for half in range(2):
                    bk = (0, 1, 7)[(2 * sub + half) % 3]
                    pY = psv(bk, [128, 512])
                    for f in range(8):
                        P.op("pe", lambda e, f=f, sub=sub, half=half, pY=pY: e.matmul(pY, lhsT=hid[:, f, sub * 128:(sub + 1) * 128], rhs=Wd_b[wb][:, f, half * 512:(half + 1) * 512],
                                                                           start=(f == 0), stop=(f == 7)), reads=["hid", D_], writes=["bank%d" % bk])
                    if half == 0:
                        P.op("dve", lambda e, pY=pY, j=j, yi=yi: e.tensor_scalar(out=y32[yi][:, 0:512], in0=pY, scalar1=gate_all[:, ex_, j:j + 1], scalar2=None, op0=ALU.mult),
                             reads=["bank%d" % bk, "gate_all"], writes=[Y_])
                    else:
                        P.op("act", lambda e, pY=pY, j=j, yi=yi: e.activation(out=y32[yi][:, 512:1024], in_=pY, func=AF.Copy, scale=gate_all[:, ex_, j:j + 1]),
                             reads=["bank%d" % bk, "gate_all"], writes=[Y_])
                P.dma("pool", lambda e, j=j, yi=yi: e.indirect_dma_start(out=out[:, :], out_offset=bass.IndirectOffsetOnAxis(ap=idx_all[:, ex_, j:j + 1], axis=0), in_=y32[yi][:, :], in_offset=None,
                                                                     compute_op=ALU.add), reads=[Y_, "idx_all", "out"], writes=["out"], chan=Y_)
                pop(1)

        for it_ in bg_items(0):
            it_()
        gather_ss(0)
        transp_ss(0)
        bg = []
        for g in range(NG):
            ex_, ssi = g // NSS, g % NSS
            if ssi == 0:
                while bg:
                    bg.pop(0)()
                bg = bg_items(ex_ + 1) if ex_ < 15 else []
            moe_ss(g, bg)
        while bg:
            bg.pop(0)()
        P.barrier()
        P.emit(st)
    return nc, P


def make_in_maps(inputs):
    x = np.asarray(inputs["x"], np.float32)
    w_in = np.asarray(inputs["w_in"], np.float32)[0]
    perm = np.concatenate([np.arange(0, 2048), np.arange(2560, 3072), np.arange(2048, 2560), np.arange(3072, 4096)])
    w_in_sw = np.ascontiguousarray(w_in[:, perm])
    wq = np.asarray(inputs["attn_q_norm_w"], np.float32)[0]
    wk = np.asarray(inputs["attn_k_norm_w"], np.float32)[0]
    wq2 = np.ascontiguousarray(np.stack([np.tile(wq, 2), np.tile(wk, 2)], axis=1))
    lbf = np.asarray(inputs["hgrn_lb_fwd"], np.float32).reshape(2, 4, 128)
    lbb = np.asarray(inputs["hgrn_lb_bwd"], np.float32).reshape(2, 4, 128)

    def lbs_of(a, b):
        t = np.stack([a, b], axis=0)
        return np.ascontiguousarray(t.transpose(3, 0, 1, 2))
    pp = np.arange(128)[:, None, None]
    kt = np.arange(2)[None, :, None]
    qq = np.arange(128)[None, None, :]
    delta = np.abs(128 * kt + pp - 64 - qq).astype(np.float32)
    band = (delta <= 64).astype(np.float32)
    em = np.zeros((128, 24, 256), np.float32)
    for h in range(8):
        slope = 2.0 ** (-8.0 * (h + 1) / 8)
        for pi, r in enumerate((1, 4, 16)):
            em[:, h * 3 + pi, :] = (np.exp(-slope * r * delta) * band).reshape(128, 256)
    ss_, tt_ = np.arange(128)[:, None], np.arange(128)[None, :]
    same = (ss_ // 64) == (tt_ // 64)
    hm = np.stack([(same & (ss_ <= tt_)), (same & (ss_ >= tt_))], axis=1).astype(np.float32)
    tt = (np.arange(32)[None, :] * 128 + np.arange(128)[:, None])
    tokab = np.stack([tt // 64, tt % 64, np.ones_like(tt)], axis=2).astype(np.float32)
    dumv = (4096 + np.arange(128)).astype(np.float32)
    common = {
        "tokab": np.ascontiguousarray(tokab),
        "iot": np.ascontiguousarray(np.broadcast_to(np.arange(1024, dtype=np.float32), (128, 1024))),
        "dum": np.ascontiguousarray(np.stack([dumv, -dumv], axis=1)),
        "emask": em,
        "hmask": np.ascontiguousarray(hm),
        "n1w": np.asarray(inputs["norm1_w"], np.float32).reshape(1, D),
        "wq2": wq2,
        "onw": np.asarray(inputs["hgrn_out_norm_w"], np.float32).reshape(1, 128),
        "w_out": np.asarray(inputs["w_out"], np.float32)[0],
        "n2w": np.asarray(inputs["norm2_w"], np.float32).reshape(1, D),
        "w_r": np.asarray(inputs["w_router"], np.float32)[0],
        "w_g": np.asarray(inputs["w_expert_gate"], np.float32)[0],
        "w_u": np.asarray(inputs["w_expert_up"], np.float32)[0],
        "w_d": np.asarray(inputs["w_expert_down"], np.float32)[0],
    }
    maps = []
    for c in range(8):
        b, hh = c // 2, c % 2
        m = dict(common)
        if hh == 0:
            m["x"] = np.ascontiguousarray(x[b])
            m["w_in"] = w_in
            m["lbs"] = lbs_of(lbf, lbb)
        else:
            m["x"] = np.ascontiguousarray(x[b, ::-1])
            m["w_in"] = w_in_sw
            m["lbs"] = lbs_of(lbb, lbf)
        maps.append(m)
    return maps


_NC_CACHE = {}


def kernel(**inputs):
    if "nc" not in _NC_CACHE:
        _NC_CACHE["nc"] = build_nc()[0]
    nc = _NC_CACHE["nc"]
    maps = make_in_maps(inputs)
    res = run_bass_kernel_spmd(nc, maps, core_ids=list(range(8)))
    outp = np.empty((4, S, D), np.float32)
    for c in range(8):
        b, hh = c // 2, c % 2
        o = res.results[c]["out"][0:S // 2]
        if hh == 0:
            outp[b, 0:S // 2] = o
        else:
            outp[b, S // 2:] = o[::-1]
    return outp
```

```python
from contextlib import ExitStack
import numpy as np
import concourse.bass as bass
import concourse.mybir as mybir
from concourse.bass_utils import run_bass_kernel_spmd

F32 = mybir.dt.float32
BF16 = mybir.dt.bfloat16
U32 = mybir.dt.uint32
AF = mybir.ActivationFunctionType
ALU = mybir.AluOpType
AX = mybir.AxisListType

S = 8192
D = 1024
PAD = 1024
EPS = 1e-6


class Tok:
    __slots__ = ("name", "last_w", "readers", "sem", "dma_cnt", "last_dma", "phys")

    def __init__(self, name):
        self.name = name
        self.last_w = None
        self.readers = []
        self.sem = None
        self.dma_cnt = 0
        self.last_dma = None
        self.phys = None


class Op:
    __slots__ = ("eng", "fn", "deps", "is_dma", "chan", "chan_ord", "ticket", "needs_inc", "idx", "phys")


class PhysSem:
    __slots__ = ("count", "sem")

    def __init__(self):
        self.count = 0
        self.sem = None


ENGS = ("pe", "act", "dve", "pool", "sp")


class Prog:
    def __init__(self, nc):
        self.nc = nc
        self.ops = []
        self.toks = {}
        self.last_op = {e: None for e in ENGS}
        self.chans = []
        self.phys_all = []
        self.phys_free = []
        self.sw_chans = []

    def tok(self, name):
        t = self.toks.get(name)
        if t is None:
            t = Tok(name)
            self.toks[name] = t
        return t

    def _T(self, xs):
        if isinstance(xs, (str, Tok)):
            xs = [xs]
        return [self.tok(x) if isinstance(x, str) else x for x in xs]

    def _add(self, eng, fn, reads, writes, is_dma, chan):
        op = Op()
        op.eng = eng
        op.fn = fn
        op.is_dma = is_dma
        op.chan = chan
        op.ticket = None
        op.needs_inc = is_dma
        op.idx = len(self.ops)
        deps = {}

        def need(d, kind):
            if d is op:
                return
            if not is_dma and not d.is_dma and d.eng == eng:
                if eng == "pe":
                    return
            deps[d.idx] = d

        for t in self._T(reads):
            if t.last_w is not None:
                need(t.last_w, "raw")
            if t.name.startswith("bank"):
                for r in t.readers:
                    if r.eng != eng:
                        need(r, "rar")
            t.readers.append(op)
        for t in self._T(writes):
            if t.last_w is not None:
                need(t.last_w, "waw")
            for r in t.readers:
                need(r, "war")
            t.last_w = op
            t.readers = []
        op.deps = list(deps.values())
        if is_dma:
            if chan.phys is None:
                if self.phys_free and eng != "pool":
                    chan.phys = self.phys_free.pop()
                else:
                    chan.phys = PhysSem()
                    self.phys_all.append(chan.phys)
                if eng == "pool":
                    self.sw_chans.append(chan)
                else:
                    self.chans.append(chan)
            chan.phys.count += 1
            op.phys = chan.phys
            op.chan_ord = chan.phys.count
            chan.last_dma = op
        self.ops.append(op)
        self.last_op[eng] = op
        return op

    def op(self, eng, fn, reads=(), writes=()):
        return self._add(eng, fn, reads, writes, False, None)

    def dma(self, eng, fn, reads=(), writes=(), chan=None):
        chan = self.tok(chan) if isinstance(chan, str) else chan
        return self._add(eng, fn, reads, writes, True, chan)

    def barrier(self):
        lasts = [o for o in self.last_op.values() if o is not None and not o.is_dma]
        dmas = [c.last_dma for c in self.chans] + [c.last_dma for c in self.sw_chans]
        for e in ENGS:
            op = Op()
            op.eng = e
            op.fn = None
            op.is_dma = False
            op.chan = None
            op.ticket = None
            op.needs_inc = False
            op.idx = len(self.ops)
            op.deps = [o for o in lasts if o.eng != e] + dmas
            self.ops.append(op)
        for t in self.toks.values():
            t.last_w = None
            t.readers = []
        for c in self.chans:
            self.phys_free.append(c.phys)
            c.phys = None
        self.chans = []

    def emit(self, stack):
        nc = self.nc
        for op in self.ops:
            for d in op.deps:
                if not d.is_dma:
                    d.needs_inc = True
        eng_sem = {}
        counts = {e: 0 for e in ENGS}
        for e in ENGS:
            eng_sem[e] = stack.enter_context(nc.semaphore("cs_" + e))
        nch = len(self.phys_all)
        for i, ph in enumerate(self.phys_all):
            ph.sem = stack.enter_context(nc.semaphore("ch%d" % i))
        for op in self.ops:
            if op.is_dma:
                pass
            elif op.needs_inc:
                counts[op.eng] += 1
                op.ticket = counts[op.eng]
        self.counts = counts
        self.nch = nch
        block = stack.enter_context(nc.Block())

        def run(e, eng):
            seen = {}
            for op in self.ops:
                if op.eng != e:
                    continue
                for d in op.deps:
                    if d.is_dma:
                        sem, val = d.phys.sem, 16 * d.chan_ord
                    else:
                        sem, val = eng_sem[d.eng], d.ticket
                    key = id(sem)
                    if seen.get(key, 0) < val:
                        eng.wait_ge(sem, val)
                        seen[key] = val
                if op.fn is None:
                    continue
                inst = op.fn(eng)
                if op.is_dma:
                    inst.then_inc(op.phys.sem, 16)
                elif op.needs_inc:
                    inst.then_inc(eng_sem[e], 1)

        @block.tensor
        def _(eng):
            run("pe", eng)

        @block.scalar
        def _(eng):
            run("act", eng)

        @block.vector
        def _(eng):
            run("dve", eng)

        @block.gpsimd
        def _(eng):
            run("pool", eng)

        @block.sync
        def _(eng):
            run("sp", eng)


class Arena:
    def __init__(self, ap, nwords):
        self.ap = ap
        self.n = nwords
        self.off = 0

    def reset(self):
        self.off = 0

    def alloc(self, shape, dt):
        free = 1
        for s in shape[1:]:
            free *= s
        words = free if dt in (F32, U32, mybir.dt.int32) else (free + 1) // 2
        words = (words + 7) // 8 * 8
        assert self.off + words <= self.n, ("SBUF arena overflow", self.off, words, self.n)
        v = self.ap[0:shape[0], self.off:self.off + words]
        self.off += words
        if dt != F32:
            v = v.bitcast(dt)
        v = v[:, 0:free]
        if len(shape) == 3:
            v = v.rearrange("p (a b) -> p a b", a=shape[1])
        elif len(shape) == 4:
            v = v.rearrange("p (a b c) -> p a b c", a=shape[1], b=shape[2])
        return v


class Rot:
    def __init__(self, items):
        self.items = items
        self.i = 0

    def next(self):
        it = self.items[self.i % len(self.items)]
        self.i += 1
        return it


def build_nc(stop_after="F", dbg=(), nst=None, skip=""):
    nc = bass.Bass("TRN2", target_bir_lowering=False)

    def din(name, shape, dt=F32):
        return nc.dram_tensor(name, list(shape), dt, kind="ExternalInput").ap()

    def dscr(name, shape, dt):
        kind = "ExternalOutput" if name in dbg else "Internal"
        return nc.dram_tensor(name, list(shape), dt, kind=kind).ap()

    x = din("x", [S, D])
    w_in = din("w_in", [D, 4096])
    n1w = din("n1w", [1, D])
    wq2 = din("wq2", [128, 2])
    lbs = din("lbs", [128, 2, 2, 4])
    onw = din("onw", [1, 128])
    w_out = din("w_out", [D, D])
    n2w = din("n2w", [1, D])
    w_r = din("w_r", [D, 16])
    w_g = din("w_g", [16, D, D])
    w_u = din("w_u", [16, D, D])
    w_d = din("w_d", [16, D, D])
    emask = din("emask", [128, 24, 256])
    hmask = din("hmask", [128, 2, 128])
    tokab = din("tokab", [128, 32, 3])
    iot = din("iot", [128, 1024])
    dum = din("dum", [128, 2])
    out = nc.dram_tensor("out", [S // 2 + 128, D], F32, kind="ExternalOutput").ap()

    QT = dscr("QT", [4, 128, S], BF16)
    KT = dscr("KT", [4, 128, S + 2 * PAD], BF16)
    VA = dscr("VA", [S + 2 * PAD, 520], BF16)
    QB = dscr("QB", [2, 4, 128, S], BF16)
    KB = dscr("KB", [2, 4, 128, S], BF16)
    KD = dscr("KD", [2, 4, 128, 64, 128], BF16)
    HV = dscr("HV", [4, 128, 64, 128], BF16)
    GS = dscr("GS", [4, 128, 64, 128], BF16)
    EBL = dscr("EBL", [2, 4, 128, 128], F32)
    MXT = dscr("MXT", [8, 128, S], BF16)
    X1 = dscr("X1", [S, D], F32)
    H2 = dscr("H2", [S // 2 + 128, D], BF16)

    P = Prog(nc)
    with ExitStack() as st:
        NW = 53000
        arena_t = st.enter_context(nc.sbuf_tensor("arena", [128, NW], F32))
        A = Arena(arena_t, NW)
        psum_t = [st.enter_context(nc.psum_tensor("ps%d" % i, [128, 512], F32)) for i in range(8)]

        def psv(bank, shape, dt=F32, half=None):
            v = psum_t[bank][:, :]
            if half is not None:
                v = v[:, half * 256:(half + 1) * 256]
            if dt != F32:
                v = v.bitcast(dt)
            free = 1
            for s_ in shape[1:]:
                free *= s_
            v = v[:, 0:free]
            if len(shape) == 3:
                v = v.rearrange("p (a b) -> p a b", a=shape[1])
            return v[0:shape[0]]

        ident = A.alloc([128, 128], BF16)
        identf = A.alloc([128, 128], F32)
        P.op("pool", lambda e: e.memset(identf, 0.0), writes=["identf"])
        P.op("pool", lambda e: e.affine_select(out=identf, in_=identf, pattern=[[-1, 128]], compare_op=ALU.not_equal,
                                               fill=1.0, base=0, channel_multiplier=1), reads=["identf"], writes=["identf"])
        P.op("dve", lambda e: e.tensor_copy(out=ident, in_=identf), reads=["identf"], writes=["ident"])
        zeros = A.alloc([128, 2048], BF16)
        P.op("pool", lambda e: e.memset(zeros, 0.0), writes=["zeros"])
        const_end = A.off

        TS = 256
        NST = nst or (S // TS)
        WIN = A.alloc([128, 8, 4096], BF16)
        wn = A.alloc([128, D], F32)
        wqs = A.alloc([128, 2], F32)
        lbt = A.alloc([128, 2, 2, 4], F32)
        lb = A.alloc([128, 2, 4], F32)
        oml = A.alloc([128, 2, 4], F32)
        bones = A.alloc([128, 128], BF16)
        rmask = A.alloc([128, TS], F32)
        stage = [A.alloc([128, 1024], F32) for _ in range(2)]

        P.dma("sp", lambda e: e.dma_start(out=wn, in_=n1w.partition_broadcast(128)), writes=["wn"], chan="wn")
        P.dma("sp", lambda e: e.dma_start(out=wqs, in_=wq2), writes=["wqs"], chan="wqs")
        P.dma("sp", lambda e: e.dma_start(out=lbt, in_=lbs), writes=["lbt"], chan="lbt")
        P.op("dve", lambda e: e.tensor_scalar(out=wqs[:, 0:1], in0=wqs[:, 0:1], scalar1=0.125, scalar2=None, op0=ALU.mult),
             reads=["wqs"], writes=["wqs"])
        P.op("dve", lambda e: e.tensor_tensor(out=lb, in0=lbt[:, :, 1, :], in1=lbt[:, :, 0, :], op=ALU.subtract), reads=["lbt"], writes=["lb"])
        P.op("act", lambda e: e.activation(out=lb, in_=lb, func=AF.Exp), reads=["lb"], writes=["lb"])
        P.op("dve", lambda e: e.tensor_scalar(out=lb, in0=lb, scalar1=1.0, scalar2=None, op0=ALU.add), reads=["lb"], writes=["lb"])
        P.op("dve", lambda e: e.reciprocal(out=lb, in_=lb), reads=["lb"], writes=["lb"])
        P.op("dve", lambda e: e.tensor_scalar(out=oml, in0=lb, scalar1=-1.0, scalar2=1.0, op0=ALU.mult, op1=ALU.add), reads=["lb"], writes=["oml"])
        P.op("pool", lambda e: e.memset(bones, 0.0), writes=["bones"])
        P.op("pool", lambda e: e.memset(bones[0:64, 0:64], 1.0), reads=["bones"], writes=["bones"])
        P.op("pool", lambda e: e.memset(bones[64:128, 64:128], 1.0), reads=["bones"], writes=["bones"])
        P.op("pool", lambda e: e.memset(rmask, 1.0), writes=["rmask"])
        P.op("pool", lambda e: e.memset(rmask.rearrange("p (c t) -> p c t", t=64)[:, :, 0:1], 0.0), reads=["rmask"], writes=["rmask"])
        for c in range(4):
            for off in (0, PAD + S):
                P.dma("sp", lambda e, c=c, off=off: e.dma_start(out=KT[c, :, off:off + PAD], in_=zeros[:, 0:PAD]),
                      reads=["zeros"], writes=["KTpad"], chan="zp%d_%d" % (c, off))
        for off in (0, PAD + S):
            for j in range(8):
                P.dma("sp", lambda e, off=off, j=j: e.dma_start(out=VA[off + j * 128:off + (j + 1) * 128, :], in_=zeros[:, 0:520]),
                      reads=["zeros"], writes=["VApad"], chan="zv%d_%d" % (off, j))
        for k in range(8):
            for hcol in range(4):
                sg, sgn = stage[hcol % 2], "stage%d" % (hcol % 2)
                P.dma("sp", lambda e, k=k, hcol=hcol, sg=sg: e.dma_start(out=sg, in_=w_in[k * 128:(k + 1) * 128, hcol * 1024:(hcol + 1) * 1024]),
                      writes=[sgn], chan=sgn)
                eng = ("pool", "dve", "act", "dve")[hcol]
                if eng == "act":
                    P.op("act", lambda e, k=k, hcol=hcol, sg=sg: e.copy(out=WIN[:, k, hcol * 1024:(hcol + 1) * 1024], in_=sg), reads=[sgn], writes=["WIN"])
                else:
                    P.op(eng, lambda e, k=k, hcol=hcol, sg=sg: e.tensor_copy(out=WIN[:, k, hcol * 1024:(hcol + 1) * 1024], in_=sg), reads=[sgn], writes=["WIN"])

        NB = 2
        xt = [A.alloc([128, 2, D], F32) for _ in range(NB)]
        junk = A.alloc([128, D], BF16)
        ss = [A.alloc([128, 2], F32) for _ in range(NB)]
        xn = A.alloc([128, 2, D], BF16)
        hT = [A.alloc([128, 8, TS], BF16) for _ in range(NB)]
        qk_st = [A.alloc([128, 8, TS], BF16) for _ in range(NB)]
        va_st = [A.alloc([128, 2, 8, 65], BF16) for _ in range(NB)]
        qb_st = [A.alloc([128, 2, 4, TS], BF16)] * NB
        kb_st = [A.alloc([128, 2, 4, TS], BF16)] * NB
        kd_st = [A.alloc([128, 2, 2, 512], BF16)] * NB
        hv_st = [A.alloc([128, 2, 512], BF16) for _ in range(NB)]
        gs_st = [A.alloc([128, 2, 512], BF16) for _ in range(NB)]
        ebl_st = [A.alloc([128, 2, 4, 4], F32) for _ in range(NB)]
        qs_t = A.alloc([128, 4, TS], F32)
        NTMP = 4
        NT5 = 5
        sqb = [A.alloc([128, TS], BF16) for _ in range(NT5)]
        rr = [A.alloc([128, TS], F32) for _ in range(NT5)]
        t_q = [A.alloc([128, TS], F32) for _ in range(NT5)]
        t_e = [A.alloc([128, TS], F32) for _ in range(NTMP)]
        t_f = [A.alloc([128, TS], F32) for _ in range(NTMP)]
        t_lf = [A.alloc([128, TS], F32) for _ in range(NTMP)]
        t_k = [A.alloc([128, TS], F32) for _ in range(NTMP)]
        t_b = [A.alloc([128, TS], F32) for _ in range(NTMP)]
        t_eb = [A.alloc([128, TS], F32) for _ in range(NTMP)]
        t_enb = [A.alloc([128, TS], F32) for _ in range(NTMP)]
        t_kbf = [A.alloc([128, TS], F32) for _ in range(NTMP)]
        t_kdT = [A.alloc([128, TS], BF16) for _ in range(9)]
        t_g = [A.alloc([128, 512], F32) for _ in range(2)]
        for i in range(NB):
            P.op("pool", lambda e, i=i: e.memset(va_st[i], 1.0), writes=["va_st%d" % i])

        psF = Rot([(psv(b_, [128, TS]), "bank%d" % b_) for b_ in (1, 2, 3, 7)])
        DEFER = 3
        pend = []

        def defer(fn_):
            pend.append(fn_)
            while len(pend) > DEFER:
                pend.pop(0)()

        def flush():
            while pend:
                pend.pop(0)()
        psTm = Rot([(psv(4 + i, [128, 512]), "bank%d" % (4 + i)) for i in range(2)])
        psM = Rot([(psv(6, [128, TS]), "bank6")])
        psK = Rot([(psv(6, [128, 2, 128], BF16), "bank6")])
        tmp_i = [0]

        def load_x(stI):
            bi = stI % NB
            t0 = stI * TS
            P.dma("sp", lambda e, bi=bi, t0=t0: e.dma_start(out=xt[bi], in_=x[t0:t0 + TS, :].rearrange("(s p) d -> p s d", p=128)),
                  writes=["xt%d" % bi], chan="xt%d" % bi)

        def prep_tile(stI):
            bi = stI % NB
            t0 = stI * TS
            X_, H_, SS_ = "xt%d" % bi, "hT%d" % bi, "ss%d" % bi
            for s in range(2):
                P.op("act", lambda e, bi=bi, s=s: e.activation(out=junk, in_=xt[bi][:, s, :], func=AF.Square, accum_out=ss[bi][:, s:s + 1]),
                     reads=[X_], writes=["junk", SS_])
            P.op("act", lambda e, bi=bi: e.activation(out=ss[bi], in_=ss[bi], func=AF.Ln, scale=1.0 / D, bias=EPS), reads=[SS_], writes=[SS_])
            P.op("act", lambda e, bi=bi: e.activation(out=ss[bi], in_=ss[bi], func=AF.Exp, scale=-0.5), reads=[SS_], writes=[SS_])
            for s in range(2):
                P.op("dve", lambda e, bi=bi, s=s: e.scalar_tensor_tensor(out=xn[:, s, :], in0=xt[bi][:, s, :], scalar=ss[bi][:, s:s + 1],
                                                                      in1=wn, op0=ALU.mult, op1=ALU.mult),
                     reads=[X_, SS_, "wn"], writes=["xn%d" % s])

        def prep_tile_b(stI):
            bi = stI % NB
            H_ = "hT%d" % bi
            for s in range(2):
                pT = psv(0, [128, 8, 128], BF16)
                for c in range(8):
                    P.op("pe", lambda e, s=s, c=c, pT=pT: e.transpose(out=pT[:, c, :], in_=xn[:, s, c * 128:(c + 1) * 128], identity=ident),
                         reads=["xn%d" % s, "ident"], writes=["bank0"])
                P.op("act", lambda e, bi=bi, s=s, pT=pT: e.copy(out=hT[bi][:, :, s * 128:(s + 1) * 128], in_=pT),
                     reads=["bank0"], writes=[H_])

        kdp = []
        late_q = []

        def do_tile(stI):
            bi = stI % NB
            t0 = stI * TS
            X_, H_, SS_ = "xt%d" % bi, "hT%d" % bi, "ss%d" % bi

            if stI + 1 < NST:
                prep_tile(stI + 1)
            if stI + 2 < NST:
                load_x(stI + 2)

            def fm_chunk(col0):
                ps, pn = psF.next()
                for k in range(8):
                    P.op("pe", lambda e, k=k, ps=ps: e.matmul(ps, lhsT=WIN[:, k, col0:col0 + 128], rhs=hT[bi][:, k, :], start=(k == 0), stop=(k == 7)),
                         reads=["WIN", H_], writes=[pn])
                return ps, pn

            VA_, HV_, GS_ = "va_st%d" % bi, "hv_st%d" % bi, "gs_st%d" % bi

            def tm_group(s):
                for gi, col0 in enumerate((1024, 3072, 3584)):
                    ps, pn = psTm.next()
                    for k in range(8):
                        P.op("pe", lambda e, k=k, ps=ps, s=s, col0=col0: e.matmul(ps, lhsT=hT[bi][:, k, s * 128:(s + 1) * 128], rhs=WIN[:, k, col0:col0 + 512],
                                                                              start=(k == 0), stop=(k == 7)), reads=["WIN", H_], writes=[pn])
                    if gi == 0:
                        P.op("act", lambda e, ps=ps, s=s: e.copy(out=va_st[bi][:, s, :, 0:64], in_=ps.rearrange("p (h d) -> p h d", h=8)), reads=[pn], writes=[VA_])
                    elif gi == 1:
                        P.op("dve", lambda e, ps=ps, s=s: e.tensor_copy(out=hv_st[bi][:, s, :], in_=ps), reads=[pn], writes=[HV_])
                    else:
                        tg, tgn = t_g[s], "t_g%d" % s
                        P.op("act", lambda e, ps=ps, tg=tg: e.activation(out=tg, in_=ps, func=AF.Exp, scale=-1.0), reads=[pn], writes=[tgn])
                        P.op("act", lambda e, tg=tg: e.activation(out=tg, in_=tg, func=AF.Ln, bias=1.0), reads=[tgn], writes=[tgn])
                        P.op("act", lambda e, tg=tg: e.activation(out=tg, in_=tg, func=AF.Exp, scale=-1.0), reads=[tgn], writes=[tgn])
                        P.op("dve", lambda e, ps=ps, tg=tg, s=s: e.tensor_tensor(out=gs_st[bi][:, s, :], in0=ps, in1=tg, op=ALU.mult), reads=[pn, tgn], writes=[GS_])

            QK_ = "qk_st%d" % bi
            for ci in range(8):
                ps, pn = fm_chunk(ci * 128)
                ti = tmp_i[0] % NT5
                tmp_i[0] += 1
                wcol = 0 if ci < 4 else 1
                P.op("act", lambda e, ps=ps, ti=ti: e.activation(out=sqb[ti], in_=ps, func=AF.Square), reads=[pn], writes=["sqb%d" % ti])
                P.op("dve", lambda e, ps=ps, ti=ti: e.tensor_copy(out=t_q[ti], in_=ps), reads=[pn, "sqb%d" % ti], writes=["t_q%d" % ti])

                def qk_tail(ti=ti, ci=ci, wcol=wcol):
                    pm, pmn = psM.next()
                    P.op("pe", lambda e: e.matmul(pm, lhsT=bones, rhs=sqb[ti], start=True, stop=True), reads=["bones", "sqb%d" % ti], writes=[pmn])
                    P.op("act", lambda e: e.activation(out=rr[ti], in_=pm, func=AF.Ln, scale=1.0 / 64, bias=EPS), reads=[pmn], writes=["rr%d" % ti])
                    P.op("act", lambda e: e.activation(out=rr[ti], in_=rr[ti], func=AF.Exp, scale=-0.5), reads=["rr%d" % ti], writes=["rr%d" % ti])
                    P.op("dve", lambda e: e.scalar_tensor_tensor(out=qk_st[bi][:, ci, :], in0=t_q[ti], scalar=wqs[:, wcol:wcol + 1], in1=rr[ti], op0=ALU.mult, op1=ALU.mult),
                         reads=["t_q%d" % ti, "wqs", "rr%d" % ti], writes=[QK_])
                defer(qk_tail)
            while late_q:
                late_q.pop(0)()
            if stI + 1 < NST:
                prep_tile_b(stI + 1)
            H4 = range(4)
            for h in H4:
                ps, pn = fm_chunk(1536 + h * 128)
                P.op("act", lambda e, ps=ps, h=h: e.activation(out=t_e[h], in_=ps, func=AF.Exp, scale=-1.0), reads=[pn], writes=["t_e%d" % h])
                P.op("dve", lambda e, ps=ps, h=h: e.tensor_copy(out=qs_t[:, h, :], in_=ps), reads=[pn, "t_e%d" % h], writes=["qs%d" % h])
            flush()
            P.dma("sp", lambda e, bi=bi, t0=t0: e.dma_start(out=QT[:, :, t0:t0 + TS].rearrange("c p t -> p c t"), in_=qk_st[bi][:, 0:4, :]),
                  reads=[QK_], writes=["QT"], chan=QK_ + "q")
            P.dma("sp", lambda e, bi=bi, t0=t0: e.dma_start(out=KT[:, :, PAD + t0:PAD + t0 + TS].rearrange("c p t -> p c t"), in_=qk_st[bi][:, 4:8, :]),
                  reads=[QK_], writes=["KT"], chan=QK_ + "k")
            tm_group(0)
            for h in H4:
                P.op("act", lambda e, h=h: e.activation(out=t_e[h], in_=t_e[h], func=AF.Ln, bias=1.0), reads=["t_e%d" % h], writes=["t_e%d" % h])
            for h in H4:
                P.op("act", lambda e, h=h: e.activation(out=t_e[h], in_=t_e[h], func=AF.Exp, scale=-1.0), reads=["t_e%d" % h], writes=["t_e%d" % h])
            for h in H4:
                P.op("dve", lambda e, h=h: e.tensor_tensor(out=qs_t[:, h, :], in0=qs_t[:, h, :], in1=t_e[h], op=ALU.mult), reads=["qs%d" % h, "t_e%d" % h], writes=["qs%d" % h])

            QB_, KB_, KD_, EB_ = "qb_st", "kb_st", "kd_st", "ebl_st%d" % bi
            for dr in range(2):
                nm = lambda s_, h: "%s%d" % (s_, h)
                for h in H4:
                    ps, pn = fm_chunk(2048 + dr * 512 + h * 128)
                    P.op("act", lambda e, ps=ps, h=h: e.activation(out=t_e[h], in_=ps, func=AF.Exp, scale=-1.0), reads=[pn], writes=[nm("t_e", h)])
                if dr == 0:
                    tm_group(1)
                else:
                    while kdp:
                        kdp.pop(0)()
                for h in H4:
                    P.op("act", lambda e, h=h: e.activation(out=t_e[h], in_=t_e[h], func=AF.Ln, bias=1.0), reads=[nm("t_e", h)], writes=[nm("t_e", h)])
                for h in H4:
                    P.op("act", lambda e, h=h: e.activation(out=t_e[h], in_=t_e[h], func=AF.Exp, scale=-1.0), reads=[nm("t_e", h)], writes=[nm("t_e", h)])
                for h in H4:
                    P.op("dve", lambda e, h=h, dr=dr: e.tensor_scalar(out=t_f[h], in0=t_e[h], scalar1=oml[:, dr, h:h + 1], scalar2=lb[:, dr, h:h + 1], op0=ALU.mult, op1=ALU.add),
                         reads=[nm("t_e", h), "oml", "lb"], writes=[nm("t_f", h)])
                for h in H4:
                    P.op("act", lambda e, h=h: e.activation(out=t_lf[h], in_=t_f[h], func=AF.Ln), reads=[nm("t_f", h)], writes=[nm("t_lf", h)])
                    P.op("pool", lambda e, h=h: e.tensor_scalar(out=t_k[h], in0=t_f[h], scalar1=-1.0, scalar2=1.0, op0=ALU.mult, op1=ALU.add), reads=[nm("t_f", h)], writes=[nm("t_k", h)])
                for h in H4:
                    P.op("dve", lambda e, h=h: e.tensor_tensor_scan(out=t_b[h], data0=rmask, data1=t_lf[h], initial=0.0, op0=ALU.mult, op1=ALU.add),
                         reads=["rmask", nm("t_lf", h)], writes=[nm("t_b", h)])
                if dr == 1:
                    for h in H4:
                        b3 = t_b[h].rearrange("p (c t) -> p c t", t=64)
                        P.op("dve", lambda e, h=h: e.tensor_tensor(out=t_e[h], in0=t_lf[h], in1=t_b[h], op=ALU.subtract), reads=[nm("t_lf", h), nm("t_b", h)], writes=[nm("t_e", h)])
                        P.op("dve", lambda e, h=h, b3=b3: e.tensor_tensor(out=b3, in0=t_e[h].rearrange("p (c t) -> p c t", t=64), in1=b3[:, :, 63:64].to_broadcast([128, 4, 64]), op=ALU.add),
                             reads=[nm("t_e", h), nm("t_b", h)], writes=[nm("t_b", h)])
                for h in H4:
                    P.op("act", lambda e, h=h: e.activation(out=t_eb[h], in_=t_b[h], func=AF.Exp), reads=[nm("t_b", h)], writes=[nm("t_eb", h)])
                for h in H4:
                    P.op("act", lambda e, h=h: e.activation(out=t_enb[h], in_=t_b[h], func=AF.Exp, scale=-1.0), reads=[nm("t_b", h)], writes=[nm("t_enb", h)])
                for h in H4:
                    P.op("pool", lambda e, h=h, dr=dr: e.tensor_tensor(out=qb_st[bi][:, dr, h, :], in0=qs_t[:, h, :], in1=t_eb[h], op=ALU.mult), reads=["qs%d" % h, nm("t_eb", h)], writes=[QB_])
                    P.op("pool", lambda e, h=h: e.tensor_tensor(out=t_kbf[h], in0=t_k[h], in1=t_enb[h], op=ALU.mult), reads=[nm("t_k", h), nm("t_enb", h)], writes=[nm("t_kbf", h)])
                col = 63 if dr == 0 else 0
                for h in H4:
                    tj = tmp_i[0] % 9
                    tmp_i[0] += 1
                    eb3 = t_eb[h].rearrange("p (c t) -> p c t", t=64)
                    P.op("dve", lambda e, h=h, dr=dr: e.tensor_copy(out=kb_st[bi][:, dr, h, :], in_=t_kbf[h]), reads=[nm("t_kbf", h)], writes=[KB_])
                    P.op("dve", lambda e, h=h, tj=tj, eb3=eb3, col=col: e.tensor_tensor(out=t_kdT[tj].rearrange("p (c t) -> p c t", t=64), in0=t_kbf[h].rearrange("p (c t) -> p c t", t=64),
                                                                         in1=eb3[:, :, col:col + 1].to_broadcast([128, 4, 64]), op=ALU.mult),
                         reads=[nm("t_kbf", h), nm("t_eb", h)], writes=["t_kdT%d" % tj])
                    P.op("pool", lambda e, h=h, dr=dr, eb3=eb3, col=col: e.tensor_copy(out=ebl_st[bi][:, dr, h, :], in_=eb3[:, :, col]), reads=[nm("t_eb", h)], writes=[EB_])

                    def kd_tail(kdT_=t_kdT[tj], dr=dr, h=h, nm_="t_kdT%d" % tj):
                        pk, pkn = psK.next()
                        for s in range(2):
                            P.op("pe", lambda e, s=s: e.transpose(out=pk[:, s, :], in_=kdT_[:, s * 128:(s + 1) * 128], identity=ident),
                                 reads=[nm_, "ident"], writes=[pkn])
                        P.op("act", lambda e: e.copy(out=kd_st[bi][:, :, dr, h * 128:(h + 1) * 128], in_=pk), reads=[pkn], writes=[KD_])
                    kdp.append(kd_tail)
            P.dma("sp", lambda e, bi=bi, t0=t0: e.dma_start(out=VA[PAD + t0:PAD + t0 + TS, :].rearrange("(s p) f -> p s f", p=128),
                                                            in_=va_st[bi].rearrange("p s h d -> p s (h d)")), reads=[VA_], writes=["VA"], chan=VA_)
            for s_ in range(2):
                P.dma("sp", lambda e, bi=bi, s_=s_: e.dma_start(out=HV[:, :, 2 * stI + s_, :].rearrange("h p k -> p h k"), in_=hv_st[bi][:, s_, :].rearrange("p (h k) -> p h k", h=4)),
                      reads=[HV_], writes=["HV"], chan=HV_ + str(s_))
            for s_ in range(2):
                P.dma("sp", lambda e, bi=bi, s_=s_: e.dma_start(out=GS[:, :, 2 * stI + s_, :].rearrange("h p k -> p h k"), in_=gs_st[bi][:, s_, :].rearrange("p (h k) -> p h k", h=4)),
                      reads=[GS_], writes=["GS"], chan=GS_ + str(s_))
            def late(stI=stI, bi=bi, t0=t0, QB_=QB_, KB_=KB_, KD_=KD_, EB_=EB_):
                while kdp:
                    kdp.pop(0)()
                for dr in range(2):
                    P.dma("sp", lambda e, dr=dr, bi=bi, t0=t0: e.dma_start(out=QB[dr, :, :, t0:t0 + TS].rearrange("h p t -> p h t"), in_=qb_st[bi][:, dr, :, :]),
                          reads=[QB_], writes=["QB"], chan=QB_ + str(dr))
                    P.dma("sp", lambda e, dr=dr, bi=bi, t0=t0: e.dma_start(out=KB[dr, :, :, t0:t0 + TS].rearrange("h p t -> p h t"), in_=kb_st[bi][:, dr, :, :]),
                          reads=[KB_], writes=["KB"], chan=KB_ + str(dr))
                    for s_ in range(2):
                        P.dma("sp", lambda e, dr=dr, bi=bi, s_=s_: e.dma_start(out=KD[dr, :, :, 2 * stI + s_, :].rearrange("h p k -> p h k"), in_=kd_st[bi][:, s_, dr, :].rearrange("p (h k) -> p h k", h=4)),
                              reads=[KD_], writes=["KD"], chan=KD_ + str(dr) + str(s_))
                    P.dma("sp", lambda e, dr=dr, bi=bi, stI=stI: e.dma_start(out=EBL[dr, :, :, stI * 4:(stI + 1) * 4].rearrange("h p c -> p h c"), in_=ebl_st[bi][:, dr, :, :]),
                          reads=[EB_], writes=["EBL"], chan=EB_ + str(dr))
            late_q.append(late)


        load_x(0)
        if NST > 1:
            load_x(1)
        prep_tile(0)
        prep_tile_b(0)
        for stI in range(NST):
            do_tile(stI)
        while late_q:
            late_q.pop(0)()

        P.barrier()
        if "DBGA" in dbg:
            for nm, ap_, shp, dt_ in (("d_xt", xt[1], [128, 2 * D], F32), ("d_ss", ss[1], [128, 2], F32), ("d_xn", xn, [128, 2 * D], BF16),
                                      ("d_hT", hT[1], [128, 8 * TS], BF16), ("d_wn", wn, [128, D], F32), ("d_win", WIN[:, 0, :], [128, 4096], BF16)):
                dd = nc.dram_tensor(nm, shp, dt_, kind="ExternalOutput").ap()
                src = ap_
                if len(ap_.shape) == 3:
                    src = ap_.rearrange("p a b -> p (a b)")
                P.dma("sp", lambda e, dd=dd, src=src: e.dma_start(out=dd, in_=src), writes=[nm], chan=nm)
            P.barrier()
        def finish():
            for j in range(4):
                P.dma("sp", lambda e, j=j: e.dma_start(out=out[j * 128:(j + 1) * 128, 0:512], in_=zeros[:, 0:1024].bitcast(F32)), reads=["zeros"], writes=["out"], chan="fin%d" % j)
            P.barrier()
            P.emit(st)

        if stop_after == "A":
            finish()
            return nc, P

        A.off = const_end
        Eb = A.alloc([128, 24, 256], BF16)
        sel = A.alloc([128, 64], F32)
        Qs = A.alloc([128, 4, 2048], BF16)
        Ks = A.alloc([128, 4, 4096], BF16)
        V1 = A.alloc([128, 17, 520], BF16)
        V4 = A.alloc([128, 4, 5, 520], BF16)
        V16 = A.alloc([128, 16, 2, 520], BF16)
        acc = A.alloc([128, 4, 2048], F32)
        att = [A.alloc([128, 2048], BF16) for _ in range(2)]
        NPT = 16
        Pt = [A.alloc([128, 256], BF16) for _ in range(NPT)]
        estg = A.alloc([128, 2048], F32)
        rden = A.alloc([128, 512], F32)
        for g in range(3):
            P.dma("sp", lambda e, g=g: e.dma_start(out=estg.rearrange("p (a b) -> p a b", a=8), in_=emask[:, g * 8:(g + 1) * 8, :]), writes=["estg"], chan="estg")
            P.op("dve", lambda e, g=g: e.tensor_copy(out=Eb[:, g * 8:(g + 1) * 8, :], in_=estg.rearrange("p (a b) -> p a b", a=8)), reads=["estg"], writes=["Eb"])
        P.op("pool", lambda e: e.memset(sel, 0.0), writes=["sel"])
        P.op("pool", lambda e: e.memset(sel[64:65, :], 1.0), reads=["sel"], writes=["sel"])
        psS = Rot([(psv(i, [128, 256]), "bank%d" % i) for i in range(4)])
        psO = Rot([(psv(4 + i, [128, 128]), "bank%d" % (4 + i)) for i in range(3)])
        psB = Rot([(psv(7, [128, 512]), "bank7")])
        pt_i = [0]
        att_i = [0]
        bpend = []

        def attn_unit2(h0, hl0, pat, r, kbase, qbase, vt0, vt1, first, vtok):
            c = h0 // 2
            pss = [psS.next() for _ in range(2)]
            for kt in range(2):
                k0 = kbase + 128 * r * kt
                for hh_ in range(2):
                    pb = 64 * hh_
                    ps, psn = pss[hh_]
                    P.op("pe", lambda e, kt=kt, k0=k0, pb=pb, ps=ps: e.matmul(ps[:, kt * 128:(kt + 1) * 128], lhsT=Ks[pb:pb + 64, c, k0:k0 + 127 * r + 1:r],
                                                                        rhs=Qs[pb:pb + 64, c, qbase:qbase + 127 * r + 1:r], start=True, stop=True),
                         reads=["Ks", "Qs"], writes=[psn])
            for hh_ in range(2):
                h, hl = h0 + hh_, hl0 + hh_
                ps, psn = pss[hh_]
                ti = pt_i[0] % NPT
                pt_i[0] += 1
                PT_ = "Pt%d" % ti
                P.op("act", lambda e, ti=ti, ps=ps: e.activation(out=Pt[ti], in_=ps, func=AF.Exp), reads=[psn], writes=[PT_])
                P.op("pool", lambda e, ti=ti, h=h: e.tensor_tensor(out=Pt[ti], in0=Pt[ti], in1=Eb[:, h * 3 + pat, :], op=ALU.mult), reads=[PT_, "Eb"], writes=[PT_])

                def pv_tail(h=h, hl=hl, ti=ti, PT_=PT_):
                    po, pon = psO.next()
                    for kt, vt in enumerate((vt0, vt1)):
                        P.op("pe", lambda e, kt=kt, vt=vt: e.matmul(po[0:65, :], lhsT=vt[:, h * 65:(h + 1) * 65], rhs=Pt[ti][:, kt * 128:(kt + 1) * 128],
                                                                  start=(kt == 0), stop=(kt == 1)), reads=[PT_, vtok], writes=[pon])
                    dst = acc[0:65, hl, qbase:qbase + 127 * r + 1:r]
                    if first:
                        P.op("dve", lambda e: e.tensor_copy(out=dst, in_=po[0:65, :]), reads=[pon], writes=["acc%d" % hl])
                    else:
                        P.op("dve", lambda e: e.tensor_tensor(out=dst, in0=dst, in1=po[0:65, :], op=ALU.add), reads=[pon, "acc%d" % hl], writes=["acc%d" % hl])
                bpend.append(pv_tail)
            while len(bpend) > 14:
                bpend.pop(0)()

        def attn_block(jb):
            p0 = 2048 * jb
            P.dma("sp", lambda e: e.dma_start(out=Qs, in_=QT[:, :, p0:p0 + 2048].rearrange("c p t -> p c t")), writes=["Qs"], chan="Qs")
            P.dma("sp", lambda e: e.dma_start(out=Ks, in_=KT[:, :, p0:p0 + 4096].rearrange("c p t -> p c t")), writes=["Ks"], chan="Ks")
            r0 = PAD + p0 - 64
            P.dma("sp", lambda e: e.dma_start(out=V1, in_=VA[r0:r0 + 17 * 128, :].rearrange("(m p) f -> p m f", p=128)), writes=["V1"], chan="V1")
            for i in range(4):
                r4 = PAD + p0 + i - 256
                P.dma("sp", lambda e, i=i, r4=r4: e.dma_start(out=V4[:, i, :, :], in_=VA[r4:r4 + 639 * 4 + 1:4, :].rearrange("(m p) f -> p m f", p=128)),
                      writes=["V4_%d" % i], chan="V4_%d" % i)
            for i in range(16):
                r16 = PAD + p0 + i - 1024
                P.dma("sp", lambda e, i=i, r16=r16: e.dma_start(out=V16[:, i, :, :], in_=VA[r16:r16 + 255 * 16 + 1:16, :].rearrange("(m p) f -> p m f", p=128)),
                      writes=["V16_%d" % i], chan="V16_%d" % i)
            for hg in range(2):
              for hl0 in (0, 2):
                h0 = hg * 4 + hl0
                for j in range(16):
                    attn_unit2(h0, hl0, 0, 1, 1024 - 64 + 128 * j, 128 * j, V1[:, j, :], V1[:, j + 1, :], True, "V1")
                for i in range(4):
                    for j in range(4):
                        attn_unit2(h0, hl0, 1, 4, 1024 + i - 256 + 512 * j, i + 512 * j, V4[:, i, j, :], V4[:, i, j + 1, :], False, "V4_%d" % i)
                for i in range(16):
                    attn_unit2(h0, hl0, 2, 16, i, i, V16[:, i, 0, :], V16[:, i, 1, :], False, "V16_%d" % i)
                while bpend:
                    bpend.pop(0)()
                for hl in (hl0, hl0 + 1):
                    h = hg * 4 + hl
                    ai = att_i[0] % 2
                    att_i[0] += 1
                    AT_ = "att%d" % ai
                    for q4 in range(4):
                        pbk, pbn = psB.next()
                        P.op("pe", lambda e, pbk=pbk, q4=q4, hl=hl: e.matmul(pbk[0:64, :], lhsT=sel[0:65, :], rhs=acc[0:65, hl, q4 * 512:(q4 + 1) * 512], start=True, stop=True),
                             reads=["sel", "acc%d" % hl], writes=[pbn])
                        P.op("act", lambda e, pbk=pbk: e.activation(out=rden[0:64, :], in_=pbk[0:64, :], func=AF.Ln), reads=[pbn], writes=["rden"])
                        P.op("act", lambda e: e.activation(out=rden[0:64, :], in_=rden[0:64, :], func=AF.Exp, scale=-1.0), reads=["rden"], writes=["rden"])
                        P.op("dve", lambda e, q4=q4, hl=hl, ai=ai: e.tensor_tensor(out=att[ai][0:64, q4 * 512:(q4 + 1) * 512], in0=acc[0:64, hl, q4 * 512:(q4 + 1) * 512],
                                                                             in1=rden[0:64, :], op=ALU.mult), reads=["rden", "acc%d" % hl], writes=[AT_])
                    c, pb = h // 2, 64 * (h % 2)
                    P.dma("sp", lambda e, c=c, pb=pb, ai=ai: e.dma_start(out=MXT[c, pb:pb + 64, p0:p0 + 2048], in_=att[ai][0:64, :]), reads=[AT_], writes=["MXT"], chan=AT_)

        nblk = 4 if nst is None else max(1, (nst * TS) // 2048)
        for jb in range(nblk):
            if "B" not in skip:
                attn_block(jb)
        P.barrier()
        if stop_after == "B":
            finish()
            return nc, P

        A.off = const_end
        hm = A.alloc([128, 2, 128], F32)
        onwt = A.alloc([128, 128], F32)
        P.dma("sp", lambda e: e.dma_start(out=hm, in_=hmask), writes=["hm"], chan="hm")
        P.dma("sp", lambda e: e.dma_start(out=onwt, in_=onw.partition_broadcast(128)), writes=["onwt"], chan="onwt")
        qbT = [A.alloc([128, S], BF16) for _ in range(2)]
        kbT = [A.alloc([128, S], BF16) for _ in range(2)]
        kdt = [A.alloc([128, 64, 128], BF16) for _ in range(2)]
        vt_ = A.alloc([128, 64, 128], BF16)
        gst = A.alloc([128, 64, 128], BF16)
        eblt = A.alloc([128, 2, 128], F32)
        oacc = A.alloc([128, 64, 128], F32)
        Sf = [[A.alloc([128, 128], F32) for _ in range(2)] for _ in range(2)]
        Sb = [[A.alloc([128, 128], BF16) for _ in range(2)] for _ in range(2)]
        sc = [0, 0]
        Am = [A.alloc([128, 128], BF16) for _ in range(2)]
        ssq = A.alloc([128, 64], F32)
        junk2 = A.alloc([128, 128], BF16)
        bo = [A.alloc([128, 128], F32) for _ in range(2)]
        bo16 = [A.alloc([128, 128], BF16) for _ in range(2)]
        mstage = A.alloc([128, S], BF16)
        nT = 64 if nst is None else (nst * TS) // 128

        gst2 = [gst, A.alloc([128, 64, 128], BF16)]

        def head_loads(h):
            for d in range(2):
                P.dma("sp", lambda e, d=d: e.dma_start(out=qbT[d][:, 0:nT * 128], in_=QB[d, h, :, 0:nT * 128]), writes=["qbT%d" % d], chan="qbT%d" % d)
                P.dma("sp", lambda e, d=d: e.dma_start(out=kbT[d][:, 0:nT * 128], in_=KB[d, h, :, 0:nT * 128]), writes=["kbT%d" % d], chan="kbT%d" % d)
                P.dma("sp", lambda e, d=d: e.dma_start(out=kdt[d][:, 0:nT, :], in_=KD[d, h, :, 0:nT, :]),
                      writes=["kdt%d" % d], chan="kdt%d" % d)
            P.dma("sp", lambda e: e.dma_start(out=vt_[:, 0:nT, :], in_=HV[h, :, 0:nT, :]), writes=["vt"], chan="vt")
            P.dma("sp", lambda e: e.dma_start(out=gst2[h % 2][:, 0:nT, :], in_=GS[h, :, 0:nT, :]), writes=["gst%d" % (h % 2)], chan="gst%d" % (h % 2))
            P.dma("sp", lambda e: e.dma_start(out=eblt[:, :, 0:2 * nT], in_=EBL[:, h, :, 0:2 * nT].rearrange("d p c -> p d c")), writes=["eblt"], chan="eblt")

        def hgrn_head(h):
            gst = gst2[h % 2]
            GST_ = "gst%d" % (h % 2)
            for d in range(2):
                sc[d] = 0
                P.op("pool", lambda e, d=d: e.memset(Sf[d][0], 0.0), writes=["Sf%d_0" % d])
                P.op("pool", lambda e, d=d: e.memset(Sb[d][0], 0.0), writes=["Sb%d_0" % d])

            def step(stp):
                Ts = (stp, nT - 1 - stp)
                halves = ((0, 1), (1, 0))
                pA = [psv(d, [128, 128]) for d in range(2)]
                pU = [[psv(2 + 2 * d + i, [128, 128]) for i in range(2)] for d in range(2)]
                pO = [psv(6 + d, [128, 128]) for d in range(2)]
                for d in range(2):
                    T = Ts[d]
                    P.op("pe", lambda e, d=d, T=T: e.matmul(pA[d], lhsT=kbT[d][:, T * 128:(T + 1) * 128], rhs=qbT[d][:, T * 128:(T + 1) * 128], start=True, stop=True),
                         reads=["kbT%d" % d, "qbT%d" % d], writes=["bank%d" % d])
                for d in range(2):
                    T = Ts[d]
                    for i, hc in enumerate(halves[d]):
                        P.op("pe", lambda e, d=d, T=T, i=i, hc=hc: e.matmul(pU[d][i], lhsT=kdt[d][hc * 64:(hc + 1) * 64, T, :], rhs=vt_[hc * 64:(hc + 1) * 64, T, :], start=True, stop=True),
                             reads=["kdt%d" % d, "vt"], writes=["bank%d" % (2 + 2 * d + i)])
                for d in range(2):
                    P.op("dve", lambda e, d=d: e.tensor_tensor(out=Am[d], in0=pA[d], in1=hm[:, d, :], op=ALU.mult), reads=["bank%d" % d, "hm"], writes=["Am%d" % d])
                for i in range(2):
                    for d in range(2):
                        T = Ts[d]
                        hc = halves[d][i]
                        P.op("pe", lambda e, d=d, T=T, hc=hc: e.matmul(pO[d][hc * 64:(hc + 1) * 64, :], lhsT=Am[d][hc * 64:(hc + 1) * 64, hc * 64:(hc + 1) * 64],
                                                                     rhs=vt_[hc * 64:(hc + 1) * 64, T, :], start=True, stop=False),
                             reads=["Am%d" % d, "vt"], writes=["bank%d" % (6 + d)])
                        c0 = T * 128 + hc * 64
                        cu, nx = sc[d] % 2, (sc[d] + 1) % 2
                        sc[d] += 1
                        P.op("pe", lambda e, d=d, hc=hc, c0=c0, i=i, cu=cu: e.matmul(pO[d][hc * 64:(hc + 1) * 64, :], lhsT=qbT[d][:, c0:c0 + 64], rhs=Sb[d][cu], start=False, stop=True),
                             reads=["qbT%d" % d, "Sb%d_%d" % (d, cu)], writes=["bank%d" % (6 + d)])
                        ch = 2 * T + hc
                        P.op("dve", lambda e, d=d, i=i, ch=ch, cu=cu, nx=nx: e.scalar_tensor_tensor(out=Sf[d][nx], in0=Sf[d][cu], scalar=eblt[:, d, ch:ch + 1], in1=pU[d][i], op0=ALU.mult, op1=ALU.add),
                             reads=["Sf%d_%d" % (d, cu), "eblt", "bank%d" % (2 + 2 * d + i)], writes=["Sf%d_%d" % (d, nx)])
                        P.op("act", lambda e, d=d, nx=nx: e.copy(out=Sb[d][nx], in_=Sf[d][nx]), reads=["Sf%d_%d" % (d, nx)], writes=["Sb%d_%d" % (d, nx)])
                for d in range(2):
                    T = Ts[d]
                    if stp < nT // 2:
                        P.op("act", lambda e, d=d, T=T: e.copy(out=oacc[:, T, :], in_=pO[d]), reads=["bank%d" % (6 + d)], writes=["oacc%d" % T])
                    else:
                        P.op("dve", lambda e, d=d, T=T: e.tensor_tensor(out=oacc[:, T, :], in0=oacc[:, T, :], in1=pO[d], op=ALU.add),
                             reads=["bank%d" % (6 + d), "oacc%d" % T], writes=["oacc%d" % T])

            for stp in range(nT):
                step(stp)
            if h + 1 < 4:
                head_loads(h + 1)
            for T in range(nT):
                P.op("act", lambda e, T=T: e.activation(out=junk2, in_=oacc[:, T, :], func=AF.Square, accum_out=ssq[:, T:T + 1]), reads=["oacc%d" % T], writes=["junk2", "ssq"])
            P.op("act", lambda e: e.activation(out=ssq[:, 0:nT], in_=ssq[:, 0:nT], func=AF.Ln, scale=1.0 / 128, bias=EPS), reads=["ssq"], writes=["ssq"])
            P.op("act", lambda e: e.activation(out=ssq[:, 0:nT], in_=ssq[:, 0:nT], func=AF.Exp, scale=-0.5), reads=["ssq"], writes=["ssq"])
            for T in range(nT):
                i2 = T % 2
                P.op("dve", lambda e, T=T, i2=i2: e.scalar_tensor_tensor(out=bo[i2], in0=oacc[:, T, :], scalar=ssq[:, T:T + 1], in1=onwt, op0=ALU.mult, op1=ALU.mult),
                     reads=["oacc%d" % T, "ssq", "onwt"], writes=["bo%d" % i2])
                P.op("pool", lambda e, T=T, i2=i2: e.tensor_tensor(out=bo16[i2], in0=bo[i2], in1=gst[:, T, :], op=ALU.mult), reads=["bo%d" % i2, GST_], writes=["bo16_%d" % i2])
                pT = psv(i2, [128, 128], BF16)
                P.op("pe", lambda e, i2=i2, pT=pT: e.transpose(out=pT, in_=bo16[i2], identity=ident), reads=["bo16_%d" % i2, "ident"], writes=["bank%d" % i2])
                P.op("act", lambda e, T=T, pT=pT: e.copy(out=mstage[:, T * 128:(T + 1) * 128], in_=pT), reads=["bank%d" % i2], writes=["mstage"])
            P.dma("sp", lambda e: e.dma_start(out=MXT[4 + h, :, 0:nT * 128], in_=mstage[:, 0:nT * 128]), reads=["mstage"], writes=["MXT"], chan="mstage")

        head_loads(0)
        for h in range(4):
            hgrn_head(h)
        P.barrier()
        if stop_after == "C":
            finish()
            return nc, P

        A.off = const_end
        nL = nT // 2
        CAPK = float(nT * 128 // 8)
        AFF = A.alloc([128, 64, 16], F32)
        posm = A.alloc([128, 32, 16], F32)
        nposm = A.alloc([128, 32, 16], F32)
        RT = A.alloc([128, 32, 16, 5], BF16)
        IOT = A.alloc([128, 1024], F32)
        idx_all = A.alloc([128, 16, 8], mybir.dt.int32)
        gate_all = A.alloc([128, 16, 8], F32)
        dumt = A.alloc([128, 2], F32)
        ones_b = A.alloc([128, 128], F32)
        P.op("pool", lambda e: e.memset(ones_b, 1.0), writes=["ones_b"])
        persist_end = A.off
        persistF_end = A.off
        WO = A.alloc([128, 8, 1024], BF16)
        WR = A.alloc([128, 8, 16], BF16)
        wrs = A.alloc([128, 8, 16], F32)
        n2t = A.alloc([128, D], F32)
        wst = [A.alloc([128, 2048], F32) for _ in range(2)]
        xt2 = [A.alloc([128, D], F32) for _ in range(2)]
        mx = [A.alloc([128, 8, 128], BF16) for _ in range(2)]
        x1t = [A.alloc([128, D], F32) for _ in range(2)]
        h2 = [A.alloc([128, D], BF16) for _ in range(2)]
        h2T = [A.alloc([128, 8, 128], BF16) for _ in range(2)]
        junk3 = A.alloc([128, D], BF16)
        ss2 = [A.alloc([128, 1], F32) for _ in range(2)]
        rmx = [A.alloc([128, 1], F32) for _ in range(2)]
        rsm = [A.alloc([128, 1], F32) for _ in range(2)]
        ex = [A.alloc([128, 16], F32) for _ in range(2)]
        P.dma("sp", lambda e: e.dma_start(out=n2t, in_=n2w.partition_broadcast(128)), writes=["n2t"], chan="n2t")
        P.dma("sp", lambda e: e.dma_start(out=wrs, in_=w_r.rearrange("(c p) e -> p c e", p=128)), writes=["wrs"], chan="wrs")
        P.op("dve", lambda e: e.tensor_copy(out=WR, in_=wrs), reads=["wrs"], writes=["WR"])
        for c2 in range(4):
            sg, sgn = wst[c2 % 2], "wst%d" % (c2 % 2)
            P.dma("sp", lambda e, c2=c2, sg=sg: e.dma_start(out=sg.rearrange("p (a b) -> p a b", a=2), in_=w_out[c2 * 256:(c2 + 1) * 256, :].rearrange("(a p) n -> p a n", p=128)),
                  writes=[sgn], chan=sgn)
            P.op("dve" if c2 % 2 else "pool", lambda e, c2=c2, sg=sg: e.tensor_copy(out=WO[:, 2 * c2:2 * c2 + 2, :], in_=sg.rearrange("p (a b) -> p a b", a=2)),
                 reads=[sgn], writes=["WO"])
        if "B" in skip:
            for c in range(4):
                for q in range(0, nT * 128, 2048):
                    w_ = min(2048, nT * 128 - q)
                    P.dma("sp", lambda e, c=c, q=q, w_=w_: e.dma_start(out=MXT[c, :, q:q + w_], in_=zeros[:, 0:w_]), reads=["zeros"], writes=["MXT"], chan="zmx")

        P.dma("sp", lambda e: e.dma_start(out=H2[S // 2:S // 2 + 128, :], in_=zeros[:, 0:1024]), reads=["zeros"], writes=["H2pad"], chan="h2pad")
        P.dma("sp", lambda e: e.dma_start(out=out[S // 2:S // 2 + 128, :], in_=zeros[:, 0:2048].bitcast(F32)), reads=["zeros"], writes=["outpad"], chan="outpad")

        if nst is not None:
            for r0 in range(nL * 128, S // 2, 128):
                P.dma("sp", lambda e, r0=r0: e.dma_start(out=H2[r0:r0 + 128, :], in_=zeros[:, 0:1024]), reads=["zeros"], writes=["H2pad"], chan="h2pad")
                P.dma("sp", lambda e, r0=r0: e.dma_start(out=out[r0:r0 + 128, :], in_=zeros[:, 0:2048].bitcast(F32)), reads=["zeros"], writes=["outpad"], chan="outpad")

        def d_load(T):
            bi = T % 2
            P.dma("sp", lambda e: e.dma_start(out=xt2[bi], in_=x[T * 128:(T + 1) * 128, :]), writes=["xt2_%d" % bi], chan="xt2_%d" % bi)
            P.dma("sp", lambda e: e.dma_start(out=mx[bi], in_=MXT[:, :, T * 128:(T + 1) * 128].rearrange("c p t -> p c t")), reads=["MXT"], writes=["mx%d" % bi], chan="mx%d" % bi)

        def d_tile(T):
            bi = T % 2
            X_, M_, X1_, H2_, HT_ = "xt2_%d" % bi, "mx%d" % bi, "x1t%d" % bi, "h2_%d" % bi, "h2T%d" % bi
            for half in range(2):
                pX = psv(2 * bi + half, [128, 512])
                for m in range(8):
                    P.op("pe", lambda e, half=half, m=m, pX=pX: e.matmul(pX, lhsT=mx[bi][:, m, :], rhs=WO[:, m, half * 512:(half + 1) * 512], start=(m == 0), stop=(m == 7)),
                         reads=[M_, "WO"], writes=["bank%d" % (2 * bi + half)])
                P.op("dve", lambda e, half=half, pX=pX: e.tensor_tensor(out=x1t[bi][:, half * 512:(half + 1) * 512], in0=xt2[bi][:, half * 512:(half + 1) * 512], in1=pX, op=ALU.add),
                     reads=[X_, "bank%d" % (2 * bi + half)], writes=[X1_])
            if T + 2 < nT:
                d_load(T + 2)
            if T < nL:
                P.dma("sp", lambda e: e.dma_start(out=out[T * 128:(T + 1) * 128, :], in_=x1t[bi]), reads=[X1_], writes=["out"], chan=X1_)
            S2_ = "ss2_%d" % bi
            P.op("act", lambda e: e.activation(out=junk3, in_=x1t[bi], func=AF.Square, accum_out=ss2[bi]), reads=[X1_], writes=["junk3", S2_])
            P.op("act", lambda e: e.activation(out=ss2[bi], in_=ss2[bi], func=AF.Ln, scale=1.0 / D, bias=EPS), reads=[S2_], writes=[S2_])
            P.op("act", lambda e: e.activation(out=ss2[bi], in_=ss2[bi], func=AF.Exp, scale=-0.5), reads=[S2_], writes=[S2_])
            P.op("dve", lambda e: e.scalar_tensor_tensor(out=h2[bi], in0=x1t[bi], scalar=ss2[bi][:, 0:1], in1=n2t, op0=ALU.mult, op1=ALU.mult),
                 reads=[X1_, S2_, "n2t"], writes=[H2_])
            if T < nL:
                P.dma("sp", lambda e: e.dma_start(out=H2[T * 128:(T + 1) * 128, :], in_=h2[bi]), reads=[H2_], writes=["H2"], chan=H2_)

        def d_tile2(T):
            bi = T % 2
            X_, M_, X1_, H2_, HT_ = "xt2_%d" % bi, "mx%d" % bi, "x1t%d" % bi, "h2_%d" % bi, "h2T%d" % bi
            pT = psv(4 + bi, [128, 8, 128], BF16)
            for c in range(8):
                P.op("pe", lambda e, c=c: e.transpose(out=pT[:, c, :], in_=h2[bi][:, c * 128:(c + 1) * 128], identity=ident), reads=[H2_, "ident"], writes=["bank%d" % (4 + bi)])
            P.op("act", lambda e: e.copy(out=h2T[bi], in_=pT), reads=["bank%d" % (4 + bi)], writes=[HT_])
            pR = psv(6 + bi, [128, 16])
            for c in range(8):
                P.op("pe", lambda e, c=c: e.matmul(pR, lhsT=h2T[bi][:, c, :], rhs=WR[:, c, :], start=(c == 0), stop=(c == 7)), reads=[HT_, "WR"], writes=["bank%d" % (6 + bi)])
            R_ = "rt%d" % bi
            P.op("dve", lambda e: e.tensor_reduce(out=rmx[bi], in_=pR, op=ALU.max, axis=AX.X), reads=["bank%d" % (6 + bi)], writes=[R_ + "m"])
            P.op("dve", lambda e: e.tensor_scalar(out=rmx[bi], in0=rmx[bi], scalar1=-1.0, scalar2=None, op0=ALU.mult), reads=[R_ + "m"], writes=[R_ + "m"])
            P.op("act", lambda e: e.activation(out=ex[bi], in_=pR, func=AF.Exp, bias=rmx[bi][:, 0:1], accum_out=rsm[bi]), reads=["bank%d" % (6 + bi), R_ + "m"], writes=[R_ + "e", R_ + "s"])
            P.op("dve", lambda e: e.reciprocal(out=rsm[bi], in_=rsm[bi]), reads=[R_ + "s"], writes=[R_ + "s"])
            P.op("dve", lambda e: e.tensor_scalar(out=AFF[:, T, :], in0=ex[bi], scalar1=rsm[bi][:, 0:1], scalar2=None, op0=ALU.mult), reads=[R_ + "e", R_ + "s"], writes=["AFF"])

        d_load(0)
        if nT > 1:
            d_load(1)
        d_tile(0)
        for T in range(nT):
            if T + 1 < nT:
                d_tile(T + 1)
            d_tile2(T)
        P.barrier()

        A.off = persist_end
        lo = A.alloc([128, 16], F32)
        hi = A.alloc([128, 16], F32)
        mid = A.alloc([128, 16], F32)
        d1 = A.alloc([128, 16], F32)
        ge = A.alloc([128, 16], F32)
        cmpb = A.alloc([128, 64, 16], BF16)
        cnt = A.alloc([128, 16], F32)
        mk = A.alloc([128, 32, 16], F32)
        P.op("pool", lambda e: e.memset(lo, 0.0), writes=["lo"])
        P.op("pool", lambda e: e.memset(hi, 1.0), writes=["hi"])
        pC = psv(0, [128, 16])
        for it in range(28):
            P.op("dve", lambda e: e.scalar_tensor_tensor(out=mid, in0=hi, scalar=0.5, in1=lo, op0=ALU.mult, op1=ALU.add), reads=["lo", "hi"], writes=["mid"])
            P.op("pool", lambda e: e.tensor_scalar(out=hi, in0=hi, scalar1=0.5, scalar2=0.0, op0=ALU.mult, op1=ALU.add), reads=["hi"], writes=["hi"])
            P.op("dve", lambda e: e.tensor_tensor(out=cmpb[:, 0:nT, :], in0=AFF[:, 0:nT, :], in1=mid.unsqueeze(1).to_broadcast([128, nT, 16]), op=ALU.is_gt),
                 reads=["AFF", "mid"], writes=["cmpb"])
            P.op("dve", lambda e: e.tensor_reduce(out=cnt, in_=cmpb[:, 0:nT, :].rearrange("p t e -> p e t"), op=ALU.add, axis=AX.X), reads=["cmpb"], writes=["cnt"])
            P.op("pe", lambda e: e.matmul(pC, lhsT=ones_b, rhs=cnt, start=True, stop=True), reads=["ones_b", "cnt"], writes=["bank0"])
            P.op("dve", lambda e: e.tensor_scalar(out=ge, in0=pC, scalar1=CAPK - 0.5, scalar2=None, op0=ALU.is_gt), reads=["bank0"], writes=["ge"])
            P.op("dve", lambda e: e.tensor_tensor(out=d1, in0=ge, in1=hi, op=ALU.mult), reads=["ge", "hi"], writes=["d1"])
            P.op("dve", lambda e: e.tensor_tensor(out=lo, in0=lo, in1=d1, op=ALU.add), reads=["lo", "d1"], writes=["lo"])
        P.op("dve", lambda e: e.tensor_tensor(out=mk[:, 0:nL, :], in0=AFF[:, 0:nL, :], in1=lo.unsqueeze(1).to_broadcast([128, nL, 16]), op=ALU.is_gt), reads=["AFF", "lo"], writes=["mk"])
        NE = nL * 16
        mkb = A.alloc([128, 32, 16], BF16)
        ustr = A.alloc([128, 128], BF16)
        ustf = A.alloc([128, 128], F32)
        onesb = A.alloc([128, 128], BF16)
        totE = A.alloc([128, 16, 32], F32)
        incE = A.alloc([128, 16, 32], F32)
        rmE = A.alloc([128, 16, 32], F32)
        pos = A.alloc([128, 32, 16], F32)
        tka = A.alloc([128, 32, 3], F32)
        ahi = A.alloc([128, 32, 16], BF16)
        ahf = A.alloc([128, 32, 16], F32)
        P.op("pool", lambda e: e.memset(ustf, 1.0), writes=["ustf"])
        P.op("pool", lambda e: e.affine_select(out=ustf, in_=ustf, pattern=[[1, 128]], compare_op=ALU.is_gt, fill=0.0, base=0, channel_multiplier=-1), reads=["ustf"], writes=["ustf"])
        P.op("dve", lambda e: e.tensor_copy(out=ustr, in_=ustf), reads=["ustf"], writes=["ustr"])
        P.op("pool", lambda e: e.memset(onesb, 1.0), writes=["onesb"])
        P.op("pool", lambda e: e.memset(rmE, 1.0), writes=["rmE"])
        P.op("pool", lambda e: e.memset(rmE[:, :, 0:1], 0.0), reads=["rmE"], writes=["rmE"])
        P.op("pool", lambda e: e.memset(totE, 0.0), writes=["totE"])
        P.dma("sp", lambda e: e.dma_start(out=tka, in_=tokab), writes=["tka"], chan="tka")
        P.dma("sp", lambda e: e.dma_start(out=IOT, in_=iot), writes=["IOT"], chan="IOT")
        P.dma("sp", lambda e: e.dma_start(out=dumt, in_=dum), writes=["dumt"], chan="dumt")
        P.op("dve", lambda e: e.tensor_copy(out=mkb[:, 0:nL, :], in_=mk[:, 0:nL, :]), reads=["mk"], writes=["mkb"])
        pW = psv(1, [128, 32, 16])
        pTt = psv(2, [128, 32, 16])
        P.op("pe", lambda e: e.matmul(pW[:, 0:nL, :], lhsT=ustr, rhs=mkb[:, 0:nL, :], start=True, stop=True), reads=["ustr", "mkb"], writes=["bank1"])
        P.op("pe", lambda e: e.matmul(pTt[:, 0:nL, :], lhsT=onesb, rhs=mkb[:, 0:nL, :], start=True, stop=True), reads=["onesb", "mkb"], writes=["bank2"])
        P.op("dve", lambda e: e.tensor_copy(out=totE[:, :, 0:nL].rearrange("p e t -> p t e"), in_=pTt[:, 0:nL, :]), reads=["bank2", "totE"], writes=["totE"])
        P.op("dve", lambda e: e.tensor_tensor_scan(out=incE.rearrange("p e t -> p (e t)"), data0=rmE.rearrange("p e t -> p (e t)"), data1=totE.rearrange("p e t -> p (e t)"),
                                                   initial=0.0, op0=ALU.mult, op1=ALU.add), reads=["rmE", "totE"], writes=["incE"])
        P.op("dve", lambda e: e.tensor_tensor(out=incE, in0=incE, in1=totE, op=ALU.subtract), reads=["incE", "totE"], writes=["incE"])
        P.op("dve", lambda e: e.tensor_tensor(out=pos[:, 0:nL, :], in0=pW[:, 0:nL, :], in1=incE[:, :, 0:nL].rearrange("p e t -> p t e"), op=ALU.add), reads=["bank1", "incE"], writes=["pos"])
        P.op("dve", lambda e: e.scalar_tensor_tensor(out=posm[:, 0:nL, :], in0=pos[:, 0:nL, :], scalar=1.0, in1=mk[:, 0:nL, :], op0=ALU.add, op1=ALU.mult), reads=["pos", "mk"], writes=["posm"])
        P.op("dve", lambda e: e.tensor_scalar(out=nposm[:, 0:nL, :], in0=posm[:, 0:nL, :], scalar1=-1.0, scalar2=1.0, op0=ALU.mult, op1=ALU.add), reads=["posm"], writes=["nposm"])
        for k3 in range(3):
            P.op("dve", lambda e, k3=k3: e.tensor_copy(out=RT[:, 0:nL, :, k3], in_=tka[:, 0:nL, k3:k3 + 1].to_broadcast([128, nL, 16])), reads=["tka", "RT"], writes=["RT"])
        P.op("dve", lambda e: e.tensor_copy(out=ahi[:, 0:nL, :], in_=AFF[:, 0:nL, :]), reads=["AFF"], writes=["ahi"])
        P.op("dve", lambda e: e.tensor_copy(out=RT[:, 0:nL, :, 3], in_=ahi[:, 0:nL, :]), reads=["ahi", "RT"], writes=["RT"])
        P.op("dve", lambda e: e.tensor_copy(out=ahf[:, 0:nL, :], in_=ahi[:, 0:nL, :]), reads=["ahi"], writes=["ahf"])
        P.op("dve", lambda e: e.tensor_tensor(out=ahf[:, 0:nL, :], in0=AFF[:, 0:nL, :], in1=ahf[:, 0:nL, :], op=ALU.subtract), reads=["AFF", "ahf"], writes=["ahf"])
        P.op("dve", lambda e: e.tensor_copy(out=RT[:, 0:nL, :, 4], in_=ahf[:, 0:nL, :]), reads=["ahf", "RT"], writes=["RT"])
        CAPS = min(1024, nL * 128)
        NJ = CAPS // 128
        SW = min(512, CAPS)
        NSS = CAPS // SW
        selt = [A.alloc([128, 1024], BF16) for _ in range(4)]
        sela = [A.alloc([128, 1024], F32) for _ in range(3)]
        idxT = [A.alloc([128, 1024], F32) for _ in range(2)]
        tab = A.alloc([128, 8, 5], F32)
        t1 = A.alloc([128, 8], F32)
        t2 = A.alloc([128, 8], F32)
        csum = A.alloc([128, 16], F32)
        pCs = psv(5, [128, 16])
        for ex_ in range(16):
            for T in range(nL):
                P.op("pe", lambda e, ex_=ex_, T=T: e.matmul(pCs[0:5, ex_:ex_ + 1], lhsT=RT[:, T, ex_, :], rhs=onesb[:, 0:1], start=(T == 0), stop=(T == nL - 1)),
                     reads=["RT", "onesb"], writes=["bank5"])
        P.op("dve", lambda e: e.tensor_copy(out=csum[0:5, :], in_=pCs[0:5, :]), reads=["bank5"], writes=["csum"])
        sel_i = [0]

        def e2_expert(ex_):
            pb_ = 2 * (ex_ % 2)
            pI = [psv(pb_ + hf, [128, SW]) for hf in range(NSS)]
            for T in range(nL):
                si = sel_i[0] % 4
                sa = sel_i[0] % 3
                sel_i[0] += 1
                SL_, SA_ = "selt%d" % si, "sela%d" % sa
                P.op("act", lambda e, T=T, sa=sa: e.activation(out=sela[sa][:, 0:CAPS], in_=IOT[:, 0:CAPS], func=AF.Square, bias=nposm[:, T, ex_:ex_ + 1], scale=1.0),
                     reads=["IOT", "nposm"], writes=[SA_])
                P.op("dve", lambda e, si=si, sa=sa: e.tensor_scalar(out=selt[si][:, 0:CAPS], in0=sela[sa][:, 0:CAPS], scalar1=1.0, scalar2=-1.0, op0=ALU.min, op1=ALU.mult),
                     reads=[SA_], writes=[SL_])
                for hf in range(NSS):
                    P.op("pe", lambda e, T=T, si=si, hf=hf: e.matmul(pI[hf][0:5, :], lhsT=RT[:, T, ex_, :], rhs=selt[si][:, hf * SW:(hf + 1) * SW], start=(T == 0), stop=(T == nL - 1)),
                         reads=["RT", SL_], writes=["bank%d" % (pb_ + hf)])
            it = ex_ % 2
            IT_ = "idxT%d" % it
            for hf in range(NSS):
                P.op("act", lambda e, hf=hf: e.activation(out=idxT[it][0:5, hf * SW:(hf + 1) * SW], in_=pI[hf][0:5, :], func=AF.Identity, bias=csum[0:5, ex_:ex_ + 1], scale=1.0),
                     reads=["bank%d" % (pb_ + hf), "csum"], writes=[IT_])
            pJ = psv(4, [128, 8, 5])
            for j in range(NJ):
                P.op("pe", lambda e, j=j: e.transpose(out=pJ[:, j, :], in_=idxT[it][0:5, j * 128:(j + 1) * 128], identity=identf[0:5, 0:5]), reads=[IT_, "identf"], writes=["bank4"])
            P.op("dve", lambda e: e.tensor_copy(out=tab[:, 0:NJ, :], in_=pJ[:, 0:NJ, :]), reads=["bank4"], writes=["tab"])
            P.op("dve", lambda e: e.scalar_tensor_tensor(out=t1[:, 0:NJ], in0=tab[:, 0:NJ, 0], scalar=64.0, in1=tab[:, 0:NJ, 1], op0=ALU.mult, op1=ALU.add), reads=["tab"], writes=["t1"])
            P.op("dve", lambda e: e.tensor_scalar(out=t2[:, 0:NJ], in0=tab[:, 0:NJ, 2], scalar1=dumt[:, 1:2], scalar2=dumt[:, 0:1], op0=ALU.mult, op1=ALU.add), reads=["tab", "dumt"], writes=["t2"])
            P.op("dve", lambda e: e.tensor_tensor(out=t1[:, 0:NJ], in0=t1[:, 0:NJ], in1=t2[:, 0:NJ], op=ALU.add), reads=["t1", "t2"], writes=["t1"])
            P.op("dve", lambda e: e.tensor_copy(out=idx_all[:, ex_, 0:NJ], in_=t1[:, 0:NJ]), reads=["t1"], writes=["idx_all"])
            P.op("dve", lambda e: e.tensor_tensor(out=gate_all[:, ex_, 0:NJ], in0=tab[:, 0:NJ, 3], in1=tab[:, 0:NJ, 4], op=ALU.add), reads=["tab"], writes=["gate_all"])

        for ex_ in range(16):
            e2_expert(ex_)
        P.barrier()

        A.off = persistF_end
        Wg_b = [A.alloc([128, 8, 1024], BF16) for _ in range(2)]
        Wu_b = [A.alloc([128, 8, 1024], BF16) for _ in range(2)]
        Wd_b = [A.alloc([128, 8, 1024], BF16) for _ in range(2)]
        wst2 = [A.alloc([128, 2048], F32) for _ in range(3)]
        xin = [A.alloc([128, D], BF16) for _ in range(8)]
        xinT = [A.alloc([128, 8, 512], BF16) for _ in range(2)]
        hid = A.alloc([128, 8, 512], BF16)
        s_g = [A.alloc([128, 512], F32) for _ in range(2)]
        y32 = [A.alloc([128, D], F32) for _ in range(3)]
        wl_i = [0]
        xin_i = [0]
        y_i = [0]
        cast_engs = ("act", "dve", "act")

        def bg_items(ex_):
            wb = ex_ % 2
            ii = ex_ % 2
            items = []

            def w_chunk(src, dstl, nm, c2):
                def f_():
                    k = wl_i[0]
                    wl_i[0] += 1
                    sg, sgn = wst2[k % 3], "wst2_%d" % (k % 3)
                    P.dma("sp", lambda e: e.dma_start(out=sg.rearrange("p (a b) -> p a b", a=2), in_=src[ex_, c2 * 256:(c2 + 1) * 256, :].rearrange("(a p) n -> p a n", p=128)),
                          writes=[sgn], chan=sgn)
                    eng = cast_engs[k % 3]
                    if eng == "act":
                        P.op("act", lambda e: e.copy(out=dstl[wb][:, 2 * c2:2 * c2 + 2, :], in_=sg.rearrange("p (a b) -> p a b", a=2)), reads=[sgn], writes=[nm])
                    else:
                        P.op(eng, lambda e: e.tensor_copy(out=dstl[wb][:, 2 * c2:2 * c2 + 2, :], in_=sg.rearrange("p (a b) -> p a b", a=2)), reads=[sgn], writes=[nm])
                return f_

            for src, dstl, nm in ((w_g, Wg_b, "Wg%d" % wb), (w_u, Wu_b, "Wu%d" % wb), (w_d, Wd_b, "Wd%d" % wb)):
                for c2 in range(4):
                    items.append(w_chunk(src, dstl, nm, c2))
            return items

        def gather_ss(g):
            ex_, ssi = g // NSS, g % NSS
            for sub in range(SW // 128):
                j = ssi * (SW // 128) + sub
                xi = (g % 2) * 4 + sub
                XI_ = "xin%d" % xi
                P.dma("pool", lambda e, j=j, xi=xi: e.indirect_dma_start(out=xin[xi][:, :], out_offset=None, in_=H2[:, :],
                                                                     in_offset=bass.IndirectOffsetOnAxis(ap=idx_all[:, ex_, j:j + 1], axis=0)),
                      reads=["H2", "idx_all"], writes=[XI_], chan=XI_)

        def transp_ss(g):
            for sub in range(SW // 128):
                xi = (g % 2) * 4 + sub
                XI_, XT_ = "xin%d" % xi, "xinT%d" % (g % 2)
                pX = psv(2, [128, 8, 128], BF16)
                for c in range(8):
                    P.op("pe", lambda e, c=c, xi=xi, pX=pX: e.transpose(out=pX[:, c, :], in_=xin[xi][:, c * 128:(c + 1) * 128], identity=ident), reads=[XI_, "ident"], writes=["bank2"])
                P.op("act", lambda e, sub=sub, pX=pX: e.copy(out=xinT[g % 2][:, :, sub * 128:(sub + 1) * 128], in_=pX), reads=["bank2"], writes=[XT_])

        NG = 16 * NSS

        def moe_ss(g, bg):
            ex_, ssi = g // NSS, g % NSS
            wb = ex_ % 2
            G_, U_, D_ = "Wg%d" % wb, "Wu%d" % wb, "Wd%d" % wb
            XT_ = "xinT%d" % (g % 2)
            xT = xinT[g % 2]
            nsub = SW // 128

            def pop(n_):
                for _ in range(n_):
                    if bg:
                        bg.pop(0)()

            if g + 1 < NG:
                gather_ss(g + 1)
            for f in range(8):
                pi = f % 2
                pG, pU = psv(3 + pi, [128, SW]), psv(5 + pi, [128, SW])
                for c in range(8):
                    P.op("pe", lambda e, c=c, f=f, pG=pG: e.matmul(pG, lhsT=Wg_b[wb][:, c, f * 128:(f + 1) * 128], rhs=xT[:, c, 0:SW], start=(c == 0), stop=(c == 7)),
                         reads=[G_, XT_], writes=["bank%d" % (3 + pi)])
                for c in range(8):
                    P.op("pe", lambda e, c=c, f=f, pU=pU: e.matmul(pU, lhsT=Wu_b[wb][:, c, f * 128:(f + 1) * 128], rhs=xT[:, c, 0:SW], start=(c == 0), stop=(c == 7)),
                         reads=[U_, XT_], writes=["bank%d" % (5 + pi)])
                S_ = "s_g%d" % pi
                P.op("act", lambda e, pi=pi, pG=pG: e.activation(out=s_g[pi][:, 0:SW], in_=pG, func=AF.Silu), reads=["bank%d" % (3 + pi)], writes=[S_])
                P.op("dve", lambda e, pi=pi, pU=pU, f=f: e.tensor_tensor(out=hid[:, f, 0:SW], in0=pU, in1=s_g[pi][:, 0:SW], op=ALU.mult), reads=["bank%d" % (5 + pi), S_], writes=["hid"])
                pop(1)
            if g + 1 < NG:
                transp_ss(g + 1)
            for sub in range(nsub):
                j = ssi * nsub + sub
                yi = y_i[0] % 3
                y_i[0] += 1
                Y_ = "y32_%d" % yi
                for half in range(2):
                    bk = (0, 1, 7)[(2 * sub + half) % 3]
                    pY = psv(bk, [128, 512])
                    for f in range(8):
                        P.op("pe", lambda e, f=f, sub=sub, half=half, pY=pY: e.matmul(pY, lhsT=hid[:, f, sub * 128:(sub + 1) * 128], rhs=Wd_b[wb][:, f, half * 512:(half + 1) * 512],
                                                                           start=(f == 0), stop=(f == 7)), reads=["hid", D_], writes=["bank%d" % bk])
                    if half == 0:
                        P.op("dve", lambda e, pY=pY, j=j, yi=yi: e.tensor_scalar(out=y32[yi][:, 0:512], in0=pY, scalar1=gate_all[:, ex_, j:j + 1], scalar2=None, op0=ALU.mult),
                             reads=["bank%d" % bk, "gate_all"], writes=[Y_])
                    else:
                        P.op("act", lambda e, pY=pY, j=j, yi=yi: e.activation(out=y32[yi][:, 512:1024], in_=pY, func=AF.Copy, scale=gate_all[:, ex_, j:j + 1]),
                             reads=["bank%d" % bk, "gate_all"], writes=[Y_])
                P.dma("pool", lambda e, j=j, yi=yi: e.indirect_dma_start(out=out[:, :], out_offset=bass.IndirectOffsetOnAxis(ap=idx_all[:, ex_, j:j + 1], axis=0), in_=y32[yi][:, :], in_offset=None,
                                                                     compute_op=ALU.add), reads=[Y_, "idx_all", "out"], writes=["out"], chan=Y_)
                pop(1)

        for it_ in bg_items(0):
            it_()
        gather_ss(0)
        transp_ss(0)
        bg = []
        for g in range(NG):
            ex_, ssi = g // NSS, g % NSS
            if ssi == 0:
                while bg:
                    bg.pop(0)()
                bg = bg_items(ex_ + 1) if ex_ < 15 else []
            moe_ss(g, bg)
        while bg:
            bg.pop(0)()
        P.barrier()
        P.emit(st)
    return nc, P


def make_in_maps(inputs):
    x = np.asarray(inputs["x"], np.float32)
    w_in = np.asarray(inputs["w_in"], np.float32)[0]
    perm = np.concatenate([np.arange(0, 2048), np.arange(2560, 3072), np.arange(2048, 2560), np.arange(3072, 4096)])
    w_in_sw = np.ascontiguousarray(w_in[:, perm])
    wq = np.asarray(inputs["attn_q_norm_w"], np.float32)[0]
    wk = np.asarray(inputs["attn_k_norm_w"], np.float32)[0]
    wq2 = np.ascontiguousarray(np.stack([np.tile(wq, 2), np.tile(wk, 2)], axis=1))
    lbf = np.asarray(inputs["hgrn_lb_fwd"], np.float32).reshape(2, 4, 128)
    lbb = np.asarray(inputs["hgrn_lb_bwd"], np.float32).reshape(2, 4, 128)

    def lbs_of(a, b):
        t = np.stack([a, b], axis=0)
        return np.ascontiguousarray(t.transpose(3, 0, 1, 2))
    pp = np.arange(128)[:, None, None]
    kt = np.arange(2)[None, :, None]
    qq = np.arange(128)[None, None, :]
    delta = np.abs(128 * kt + pp - 64 - qq).astype(np.float32)
    band = (delta <= 64).astype(np.float32)
    em = np.zeros((128, 24, 256), np.float32)
    for h in range(8):
        slope = 2.0 ** (-8.0 * (h + 1) / 8)
        for pi, r in enumerate((1, 4, 16)):
            em[:, h * 3 + pi, :] = (np.exp(-slope * r * delta) * band).reshape(128, 256)
    ss_, tt_ = np.arange(128)[:, None], np.arange(128)[None, :]
    same = (ss_ // 64) == (tt_ // 64)
    hm = np.stack([(same & (ss_ <= tt_)), (same & (ss_ >= tt_))], axis=1).astype(np.float32)
    tt = (np.arange(32)[None, :] * 128 + np.arange(128)[:, None])
    tokab = np.stack([tt // 64, tt % 64, np.ones_like(tt)], axis=2).astype(np.float32)
    dumv = (4096 + np.arange(128)).astype(np.float32)
    common = {
        "tokab": np.ascontiguousarray(tokab),
        "iot": np.ascontiguousarray(np.broadcast_to(np.arange(1024, dtype=np.float32), (128, 1024))),
        "dum": np.ascontiguousarray(np.stack([dumv, -dumv], axis=1)),
        "emask": em,
        "hmask": np.ascontiguousarray(hm),
        "n1w": np.asarray(inputs["norm1_w"], np.float32).reshape(1, D),
        "wq2": wq2,
        "onw": np.asarray(inputs["hgrn_out_norm_w"], np.float32).reshape(1, 128),
        "w_out": np.asarray(inputs["w_out"], np.float32)[0],
        "n2w": np.asarray(inputs["norm2_w"], np.float32).reshape(1, D),
        "w_r": np.asarray(inputs["w_router"], np.float32)[0],
        "w_g": np.asarray(inputs["w_expert_gate"], np.float32)[0],
        "w_u": np.asarray(inputs["w_expert_up"], np.float32)[0],
        "w_d": np.asarray(inputs["w_expert_down"], np.float32)[0],
    }
    maps = []
    for c in range(8):
        b, hh = c // 2, c % 2
        m = dict(common)
        if hh == 0:
            m["x"] = np.ascontiguousarray(x[b])
            m["w_in"] = w_in
            m["lbs"] = lbs_of(lbf, lbb)
        else:
            m["x"] = np.ascontiguousarray(x[b, ::-1])
            m["w_in"] = w_in_sw
            m["lbs"] = lbs_of(lbb, lbf)
        maps.append(m)
    return maps


_NC_CACHE = {}


def kernel(**inputs):
    if "nc" not in _NC_CACHE:
        _NC_CACHE["nc"] = build_nc()[0]
    nc = _NC_CACHE["nc"]
    maps = make_in_maps(inputs)
    res = run_bass_kernel_spmd(nc, maps, core_ids=list(range(8)))
    outp = np.empty((4, S, D), np.float32)
    for c in range(8):
        b, hh = c // 2, c % 2
        o = res.results[c]["out"][0:S // 2]
        if hh == 0:
            outp[b, 0:S // 2] = o
        else:
            outp[b, S // 2:] = o[::-1]
    return outp
```
